# Optimizing a Trainium2 kernel written in Bass

```python
import jax
import jax.numpy as jnp
from jax import lax
import numpy as np

D_MODEL = 2048
BATCH = 4
SEQ = 4096
DEPTH = 4

GRID_W = 64
CTX_LEN = 256
MIX_W = D_MODEL
FOURIER_W = D_MODEL // 4
FOURIER_GROUPS = 4
HGRN_DIM = 128
HGRN_W = D_MODEL // 4
HGRN_HEADS = HGRN_W // HGRN_DIM
CHUNK = 64
MLA_NOPE = 128
MLA_ROPE = 64
MLA_V = 128
MLA_W = MIX_W - FOURIER_W - HGRN_W
MLA_HEADS = MLA_W // MLA_V
Q_LORA = D_MODEL // 4
KV_LORA = D_MODEL // 8
MLA_SCALE = (MLA_NOPE + MLA_ROPE) ** -0.5
Q_BLOCK = 128
ROPE_BASE = 10000.0
IN_SPLITS = (FOURIER_W, HGRN_W, HGRN_W, HGRN_W, HGRN_W, HGRN_W, Q_LORA, KV_LORA, MLA_ROPE)
IN_PROJ_W = FOURIER_W + 5 * HGRN_W + Q_LORA + KV_LORA + MLA_ROPE
N_EXPERTS = 32
TOP_K = 4
EXPERT_FF = 3 * D_MODEL // 8
SWIGLU_ALPHA = 1.702
SWIGLU_LIMIT = 7.0
MOE_BLOCK = 128
N_MOD = 6
DEEPNORM_ALPHA = (2 * DEPTH) ** 0.25
DEEPNORM_BETA = (8 * DEPTH) ** -0.25
EPS = 1e-6

kernel_name = 'hybrid_fourier_hgrn2_mla_moe_dit'


def layer_norm(x):
    xf = x.astype(jnp.float32)
    mu = jnp.mean(xf, axis=-1, keepdims=True)
    var = jnp.mean(jnp.square(xf - mu), axis=-1, keepdims=True)
    return ((xf - mu) * lax.rsqrt(var + EPS)).astype(x.dtype)


def rms_norm(x, g):
    xf = x.astype(jnp.float32)
    y = xf * lax.rsqrt(jnp.mean(jnp.square(xf), axis=-1, keepdims=True) + EPS)
    return (y * g).astype(x.dtype)


def modulate(x, shift, scale):
    return layer_norm(x) * (1 + scale) + shift


def post_norm(x, update, g, b):
    return layer_norm(DEEPNORM_ALPHA * x + update) * g + b


def axial_rope(n_lat):
    rows = n_lat // GRID_W
    t = jnp.arange(rows * GRID_W)
    row, col = t // GRID_W, t % GRID_W
    n_freq = MLA_ROPE // 4
    inv = ROPE_BASE ** (-jnp.arange(n_freq, dtype=jnp.float32) / n_freq)
    ang = jnp.concatenate([row[:, None] * inv, col[:, None] * inv], axis=-1)
    return jnp.cos(ang), jnp.sin(ang)


def apply_rope(x, cos, sin):
    xp = x.reshape(*x.shape[:-1], MLA_ROPE // 2, 2)
    a, b = xp[..., 0], xp[..., 1]
    out = jnp.stack([a * cos - b * sin, a * sin + b * cos], axis=-1)
    return out.reshape(x.shape).astype(x.dtype)


def fourier_mix(u):
    B, T, _ = u.shape
    ug = u.astype(jnp.float32).reshape(B, T, FOURIER_GROUPS, FOURIER_W // FOURIER_GROUPS)
    y = jnp.fft.fft2(ug, axes=(1, 3), norm='ortho').real
    return y.reshape(B, T, FOURIER_W).astype(u.dtype)


def mla_queries(cq, q_norm_g, w_uq, rope):
    B, T, _ = cq.shape
    q = (rms_norm(cq, q_norm_g) @ w_uq).reshape(B, T, MLA_HEADS, MLA_NOPE + MLA_ROPE)
    q_nope, q_rope = q[..., :MLA_NOPE], q[..., MLA_NOPE:]
    if rope is not None:
        q_rope = apply_rope(q_rope, rope[0][:, None, :], rope[1][:, None, :])
    return q_nope, q_rope


def mla_keys(ckv, k_rope, kv_norm_g, w_ukv, rope):
    B, T, _ = ckv.shape
    kv = (rms_norm(ckv, kv_norm_g) @ w_ukv).reshape(B, T, MLA_HEADS, MLA_NOPE + MLA_V)
    k_nope, v = kv[..., :MLA_NOPE], kv[..., MLA_NOPE:]
    if rope is not None:
        k_rope = apply_rope(k_rope, rope[0], rope[1])
    return k_nope, k_rope, v


def block_attention(q_nope, q_rope, k_nope, k_rope, v):
    B, T, H, _ = q_nope.shape
    nb = T // Q_BLOCK

    def blocks(a):
        return a.reshape(B, nb, Q_BLOCK, H, a.shape[-1]).swapaxes(0, 1)

    def attend(qs):
        qn, qr = qs
        s = jnp.einsum('bqhd,bkhd->bhqk', qn, k_nope) + jnp.einsum('bqhr,bkr->bhqk', qr, k_rope)
        p = jax.nn.softmax(s.astype(jnp.float32) * MLA_SCALE, axis=-1)
        return jnp.einsum('bhqk,bkhv->bqhv', p.astype(v.dtype), v)

    o = lax.map(attend, (blocks(q_nope), blocks(q_rope)))
    return o.swapaxes(0, 1).reshape(B, T, H * MLA_V)


def hgrn_lower_bounds(logits):
    cs = jnp.cumsum(jax.nn.softmax(logits.astype(jnp.float32), axis=0), axis=0)
    return cs - cs[:1]


def hgrn_inputs(q, i, z, lb, reverse):
    B, T, _ = q.shape
    log_f = jnp.logaddexp(jnp.log(lb), jnp.log1p(-lb) + jax.nn.log_sigmoid(z.astype(jnp.float32)))
    k = -jnp.expm1(log_f)
    out = tuple(a.astype(jnp.float32).reshape(B, T, HGRN_HEADS, HGRN_DIM) for a in (q, k, i, log_f))
    if reverse:
        out = tuple(jnp.flip(a, axis=1) for a in out)
    return out


def chunk_gla(q, k, v, log_f, s0):
    B, T, H, _ = q.shape
    n = T // CHUNK

    def chunks(a):
        return a.reshape(B, n, CHUNK, H, a.shape[-1]).transpose(1, 0, 3, 2, 4)

    a = jnp.cumsum(chunks(log_f), axis=3)
    incl = jnp.tril(jnp.ones((CHUNK, CHUNK), bool))

    def step(s, blk):
        qb, kb, vb, ab = blk
        inter = jnp.einsum('bhtk,bhkv->bhtv', qb * jnp.exp(ab), s)
        rel = ab[:, :, :, None, :] - ab[:, :, None, :, :]
        decay = jnp.exp(jnp.where(incl[:, :, None], rel, -jnp.inf))
        scores = jnp.einsum('bhtk,bhsk,bhtsk->bhts', qb, kb, decay)
        intra = jnp.einsum('bhts,bhsv->bhtv', scores, vb)
        a_end = ab[:, :, -1]
        s_new = jnp.exp(a_end)[..., None] * s + jnp.einsum('bhsk,bhsv->bhkv', kb * jnp.exp(a_end[:, :, None] - ab), vb)
        return s_new, inter + intra

    s_end, o = lax.scan(step, s0, (chunks(q), chunks(k), chunks(v), a))
    return o.transpose(1, 0, 3, 2, 4).reshape(B, T, H, v.shape[-1]), s_end


def hgrn_readout(o, g, norm_g):
    B, T = o.shape[:2]
    o = rms_norm(o, norm_g.reshape(HGRN_HEADS, HGRN_DIM)).reshape(B, T, HGRN_W)
    return (o * jax.nn.silu(g.astype(jnp.float32))).astype(g.dtype)


def hgrn2_mix(parts_c, parts_l, lb, norm_g, last):
    q_c, i_c, g_c, zf_c, zb_c = parts_c
    q_l, i_l, g_l, zf_l, zb_l = parts_l
    s0 = jnp.zeros((q_l.shape[0], HGRN_HEADS, HGRN_DIM, HGRN_DIM), jnp.float32)
    o_c, o_l = [], []
    for d, (z_c, z_l) in enumerate(((zf_c, zf_l), (zb_c, zb_l))):
        rev = d == 1
        oc, s_ctx = chunk_gla(*hgrn_inputs(q_c, i_c, z_c, lb[d], rev), s0)
        ol, _ = chunk_gla(*hgrn_inputs(q_l, i_l, z_l, lb[d], rev), s_ctx)
        o_c.append(jnp.flip(oc, axis=1) if rev else oc)
        o_l.append(jnp.flip(ol, axis=1) if rev else ol)
    out_l = hgrn_readout(o_l[0] + o_l[1], g_l, norm_g)
    if last:
        return None, out_l
    return hgrn_readout(o_c[0] + o_c[1], g_c, norm_g), out_l


def token_mixers(h_c, h_l, rope, lb, w_in, q_norm_g, w_uq, kv_norm_g, w_ukv, hgrn_norm_g, w_out, last):
    idx = np.cumsum(IN_SPLITS)[:-1].tolist()
    f_c, *hg_c, cq_c, ckv_c, kr_c = jnp.split(h_c @ w_in, idx, axis=-1)
    f_l, *hg_l, cq_l, ckv_l, kr_l = jnp.split(h_l @ w_in, idx, axis=-1)
    kn_c, krot_c, v_c = mla_keys(ckv_c, kr_c, kv_norm_g, w_ukv, None)
    kn_l, krot_l, v_l = mla_keys(ckv_l, kr_l, kv_norm_g, w_ukv, rope)
    qn_l, qr_l = mla_queries(cq_l, q_norm_g, w_uq, rope)
    att_l = block_attention(qn_l, qr_l, jnp.concatenate([kn_c, kn_l], axis=1),
                            jnp.concatenate([krot_c, krot_l], axis=1), jnp.concatenate([v_c, v_l], axis=1))
    hg_out_c, hg_out_l = hgrn2_mix(hg_c, hg_l, lb, hgrn_norm_g, last)
    out_l = jnp.concatenate([fourier_mix(f_l), hg_out_l, att_l.astype(h_l.dtype)], axis=-1) @ w_out
    if last:
        return None, out_l
    qn_c, qr_c = mla_queries(cq_c, q_norm_g, w_uq, None)
    att_c = block_attention(qn_c, qr_c, kn_c, krot_c, v_c)
    out_c = jnp.concatenate([fourier_mix(f_c), hg_out_c, att_c.astype(h_c.dtype)], axis=-1) @ w_out
    return out_c, out_l


def moe_ffn(h, router_w, router_b, w1, b1, w2, b2):
    n = h.shape[0]
    logits = (h @ router_w + router_b).astype(jnp.float32)
    top_v, top_e = lax.top_k(logits, TOP_K)
    gates = jax.nn.softmax(top_v, axis=-1)
    flat_e = top_e.reshape(-1)
    order = jnp.argsort(flat_e)
    sorted_e = flat_e[order]
    counts = jnp.bincount(flat_e, length=N_EXPERTS)
    padded = (counts + MOE_BLOCK - 1) // MOE_BLOCK * MOE_BLOCK
    pad_end = jnp.cumsum(padded)
    pad_start = pad_end - padded
    start = jnp.cumsum(counts) - counts
    dest = pad_start[sorted_e] + jnp.arange(n * TOP_K) - start[sorted_e]
    m_pad = -(-n * TOP_K // MOE_BLOCK) * MOE_BLOCK + N_EXPERTS * MOE_BLOCK
    n_blocks = m_pad // MOE_BLOCK
    tok_pad = jnp.zeros((m_pad,), jnp.int32).at[dest].set((order // TOP_K).astype(jnp.int32))
    gate_pad = jnp.zeros((m_pad,), h.dtype).at[dest].set(gates.reshape(-1)[order].astype(h.dtype))
    block_e = jnp.minimum(jnp.searchsorted(pad_end, jnp.arange(n_blocks) * MOE_BLOCK, side='right'), N_EXPERTS - 1)

    def run_block(blk):
        tok, gate, e = blk
        u = h[tok] @ w1[e] + b1[e]
        x_glu = jnp.minimum(u[:, 0::2], SWIGLU_LIMIT)
        x_lin = jnp.clip(u[:, 1::2], -SWIGLU_LIMIT, SWIGLU_LIMIT)
        act = x_glu * jax.nn.sigmoid(SWIGLU_ALPHA * x_glu) * (x_lin + 1)
        return (act @ w2[e] + b2[e]) * gate[:, None]

    y = lax.map(run_block, (tok_pad.reshape(n_blocks, MOE_BLOCK), gate_pad.reshape(n_blocks, MOE_BLOCK), block_e))
    return jnp.zeros_like(h).at[tok_pad].add(y.reshape(m_pad, -1).astype(h.dtype))


def setup_inputs(seed: int = 0) -> dict:
    key = jax.random.key(seed)
    ks = jax.random.split(key, 24)
    f32 = jnp.float32

    def nrm(k, shape, scale):
        return jax.random.normal(k, shape, f32) * scale

    def gain(k, shape):
        return 1.0 + 0.02 * jax.random.normal(k, shape, f32)

    hq = MLA_HEADS * (MLA_NOPE + MLA_ROPE)
    hkv = MLA_HEADS * (MLA_NOPE + MLA_V)
    return {
        'x': nrm(ks[0], (BATCH, SEQ, D_MODEL), 1.0),
        'c': nrm(ks[1], (BATCH, D_MODEL), 1.0),
        'ctx': nrm(ks[2], (BATCH, CTX_LEN, D_MODEL), 1.0),
        'c_ctx': nrm(ks[3], (D_MODEL,), 1.0),
        'w_ada': nrm(ks[4], (DEPTH, D_MODEL, N_MOD * D_MODEL), D_MODEL ** -0.5),
        'b_ada': nrm(ks[5], (DEPTH, N_MOD * D_MODEL), 0.02),
        'w_in': nrm(ks[6], (DEPTH, D_MODEL, IN_PROJ_W), D_MODEL ** -0.5),
        'mla_q_norm': gain(ks[7], (DEPTH, Q_LORA)),
        'w_uq': nrm(ks[8], (DEPTH, Q_LORA, hq), Q_LORA ** -0.5),
        'mla_kv_norm': gain(ks[9], (DEPTH, KV_LORA)),
        'w_ukv': nrm(ks[10], (DEPTH, KV_LORA, hkv), KV_LORA ** -0.5),
        'hgrn_lb_logits': nrm(ks[11], (DEPTH, 2, HGRN_W), 0.1),
        'hgrn_norm': gain(ks[12], (DEPTH, HGRN_W)),
        'w_out': nrm(ks[13], (DEPTH, MIX_W, D_MODEL), DEEPNORM_BETA * MIX_W ** -0.5),
        'ln1_g': gain(ks[14], (DEPTH, D_MODEL)),
        'ln1_b': nrm(ks[15], (DEPTH, D_MODEL), 0.02),
        'router_w': nrm(ks[16], (DEPTH, D_MODEL, N_EXPERTS), D_MODEL ** -0.5),
        'router_b': nrm(ks[17], (DEPTH, N_EXPERTS), 0.01),
        'w1': nrm(ks[18], (DEPTH, N_EXPERTS, D_MODEL, 2 * EXPERT_FF), D_MODEL ** -0.5),
        'b1': nrm(ks[19], (DEPTH, N_EXPERTS, 2 * EXPERT_FF), 0.02),
        'w2': nrm(ks[20], (DEPTH, N_EXPERTS, EXPERT_FF, D_MODEL), DEEPNORM_BETA * EXPERT_FF ** -0.5),
        'b2': nrm(ks[21], (DEPTH, N_EXPERTS, D_MODEL), 0.02),
        'ln2_g': gain(ks[22], (DEPTH, D_MODEL)),
        'ln2_b': nrm(ks[23], (DEPTH, D_MODEL), 0.02),
    }


def reference(x, c, ctx, c_ctx, w_ada, b_ada, w_in, mla_q_norm, w_uq, mla_kv_norm, w_ukv, hgrn_lb_logits,
              hgrn_norm, w_out, ln1_g, ln1_b, router_w, router_b, w1, b1, w2, b2, ln2_g, ln2_b):
    B, n_lat, D = x.shape
    rope = axial_rope(n_lat)
    lower_bounds = hgrn_lower_bounds(hgrn_lb_logits)
    for l in range(DEPTH):
        last = l == DEPTH - 1
        mod_l = jnp.split((jax.nn.silu(c) @ w_ada[l] + b_ada[l])[:, None, :], N_MOD, axis=-1)
        mod_c = jnp.split((jax.nn.silu(c_ctx) @ w_ada[l] + b_ada[l])[None, None, :], N_MOD, axis=-1)
        h_l = modulate(x, mod_l[0], mod_l[1])
        h_c = modulate(ctx, mod_c[0], mod_c[1])
        mix_c, mix_l = token_mixers(h_c, h_l, rope, lower_bounds[l], w_in[l], mla_q_norm[l], w_uq[l],
                                    mla_kv_norm[l], w_ukv[l], hgrn_norm[l], w_out[l], last)
        x = post_norm(x, mod_l[2] * mix_l, ln1_g[l], ln1_b[l])
        h_l = modulate(x, mod_l[3], mod_l[4])
        moe_p = (router_w[l], router_b[l], w1[l], b1[l], w2[l], b2[l])
        if last:
            ffn_l = moe_ffn(h_l.reshape(-1, D), *moe_p).reshape(x.shape)
        else:
            ctx = post_norm(ctx, mod_c[2] * mix_c, ln1_g[l], ln1_b[l])
            h_c = modulate(ctx, mod_c[3], mod_c[4])
            n_ctx = ctx.shape[0] * ctx.shape[1]
            ffn_all = moe_ffn(jnp.concatenate([h_c.reshape(-1, D), h_l.reshape(-1, D)], axis=0), *moe_p)
            ctx = post_norm(ctx, mod_c[5] * ffn_all[:n_ctx].reshape(ctx.shape), ln2_g[l], ln2_b[l])
            ffn_l = ffn_all[n_ctx:].reshape(x.shape)
        x = post_norm(x, mod_l[5] * ffn_l, ln2_g[l], ln2_b[l])
    return x
```

```python
import time
import ml_dtypes

import numpy as np
from contextlib import ExitStack
import concourse.bass as bass
import concourse.mybir as mybir
from concourse.bass_utils import run_bass_kernel_spmd

F32 = mybir.dt.float32
BF16 = mybir.dt.bfloat16
ALU = mybir.AluOpType
AF = mybir.ActivationFunctionType
AX = mybir.AxisListType

SEM_CH = 4000
DMA_SLOTS = 8
DMA_USES = 240
SAME_ENG_SYNC = True
DT_SIZE = {F32: 4, BF16: 2}


class Buf:
    __slots__ = ("w", "r")

    def __init__(self):
        self.w = {}
        self.r = {}


class V:
    __slots__ = ("ap", "bufs")

    def __init__(self, ap, bufs):
        self.ap = ap
        self.bufs = bufs

    def __getitem__(self, key):
        return V(self.ap[key], self.bufs)

    def bc(self, shape):
        return V(self.ap.to_broadcast(shape), self.bufs)

    def re(self, s, **kw):
        return V(self.ap.rearrange(s, **kw), self.bufs)


class Tile:
    def __init__(self, ap, nsub=1):
        self.ap = ap
        self.nsub = nsub
        self.subs = [Buf() for _ in range(nsub)]
        self.shape = tuple(ap.shape)

    @property
    def a(self):
        return V(self.ap, self.subs)

    def __getitem__(self, key):
        return V(self.ap[key], self.subs)

    def q(self, i, w=128):
        return V(self.ap[:, i * w:(i + 1) * w], [self.subs[i]])

    def s(self, i, j=None):
        if self.nsub == 1:
            raise ValueError
        if j is None:
            return V(self.ap[:, i], [self.subs[i]])
        return V(self.ap[:, i:j], self.subs[i:j])


class Prog:
    ENGS = ("pe", "dve", "act", "pool", "sp")

    def __init__(self, nc):
        self.nc = nc
        self.es = ExitStack()
        self.ops = {e: [] for e in self.ENGS}
        self.cnt = {e: 0 for e in self.ENGS}
        self.dq = {e: 0 for e in self.ENGS}
        self.seen = {e: {} for e in self.ENGS}
        self.semkeys = set()
        self.last_dma = {}
        self.arena_bytes = 206 * 1024
        self.arena = self.es.enter_context(nc.sbuf_tensor("arena", [128, self.arena_bytes // 4], F32))
        self.top = 0
        self.ghosts = []
        self.live = []
        self.psum = []
        for i in range(8):
            t = self.es.enter_context(nc.psum_tensor(f"ps{i}", [128, 512], F32))
            self.psum.append(Tile(t[:], 4))
        self.ndram = 0

    def mark(self):
        return self.top

    def release(self, m):
        keep = []
        for (lo, hi, t) in self.live:
            if lo >= m:
                for b in t.subs:
                    self.ghosts.append((lo, hi, b))
            else:
                keep.append((lo, hi, t))
        self.live = keep
        self.top = m

    def tile(self, shape, dtype=F32, nsub=1, name=None):
        p = shape[0]
        free = int(np.prod(shape[1:]))
        nbytes = free * DT_SIZE[dtype]
        nbytes_al = (nbytes + 31) // 32 * 32
        lo = self.top
        hi = lo + nbytes_al
        if hi > self.arena_bytes:
            raise MemoryError(f"SBUF arena overflow: need {hi} for {name} {shape}")
        self.top = hi
        ap = self.arena[0:p, lo // 4:(lo + nbytes_al) // 4]
        if dtype != F32:
            ap = ap.bitcast(dtype)
        ap = ap[:, 0:free]
        if len(shape) > 2:
            names = " ".join(f"d{i}" for i in range(len(shape) - 1))
            kw = {f"d{i}": shape[i + 1] for i in range(len(shape) - 2)}
            ap = ap.rearrange(f"p ({names}) -> p {names}", **kw)
        t = Tile(ap, nsub)
        ng = []
        for (glo, ghi, b) in self.ghosts:
            if glo < hi and ghi > lo:
                for sb in t.subs:
                    for d in (b.w, b.r):
                        for k, v in d.items():
                            if sb.w.get(k, -1) < v:
                                sb.w[k] = v
                if glo < lo or ghi > hi:
                    ng.append((glo, ghi, b))
            else:
                ng.append((glo, ghi, b))
        self.ghosts = ng
        self.live.append((lo, hi, t))
        return t

    def dram(self, name, shape, dtype=F32, kind="Internal", nsub=1):
        h = self.nc.dram_tensor(name, list(shape), dtype, kind=kind)
        return Tile(h.ap(), nsub)

    def op(self, eng, fn, reads, writes, dma=False):
        toks = {}

        def add(d):
            for k, v in d.items():
                if toks.get(k, -1) < v:
                    toks[k] = v

        for v in reads:
            for b in v.bufs:
                add(b.w)
        for v in writes:
            for b in v.bufs:
                add(b.w)
                add(b.r)
        if dma:
            n = self.dq[eng]
            self.dq[eng] += 1
            j = n % DMA_SLOTS
            use = n // DMA_SLOTS
            if use > 0:
                pu = use - 1
                add({("q", f"q{eng}{j}_{pu // DMA_USES}"): 16 * (pu % DMA_USES + 1)})
            key = ("q", f"q{eng}{j}_{use // DMA_USES}")
            val = 16 * (use % DMA_USES + 1)
            self.semkeys.add(key[1])
            self.last_dma[key[1]] = val
            my = (key, val)
        else:
            i = self.cnt[eng]
            self.cnt[eng] += 1
            my = (("c", eng), i)
        deps = []
        for k, v in toks.items():
            if not dma and k == ("c", eng):
                if eng == "pe" or not SAME_ENG_SYNC:
                    continue
            deps.append((k, v))
        for v in reads:
            for b in v.bufs:
                if b.r.get(my[0], -1) < my[1]:
                    b.r[my[0]] = my[1]
        for v in writes:
            for b in v.bufs:
                b.w = {my[0]: my[1]}
                b.r = {}
        self.ops[eng].append((deps, fn, my, dma))

    def dma(self, out, in_, eng="sp"):
        self.op(eng, lambda e: e.dma_start(out=out.ap, in_=in_.ap), [in_], [out], dma=True)

    def mm(self, out, lhsT, rhs, start=True, stop=True):
        self.op("pe", lambda e: e.matmul(out.ap, lhsT.ap, rhs.ap, start=start, stop=stop),
                [lhsT, rhs], [out])

    def transpose(self, out, in_, ident):
        self.op("pe", lambda e: e.transpose(out.ap, in_.ap, ident.ap), [in_, ident], [out])

    def act(self, out, in_, func, bias=None, scale=None, accum_out=None, eng="act"):
        reads = [in_]
        kw = {}
        if bias is not None:
            if isinstance(bias, V):
                reads.append(bias)
                kw["bias"] = bias.ap
            else:
                kw["bias"] = float(bias)
        if scale is not None:
            if isinstance(scale, V):
                reads.append(scale)
                kw["scale"] = scale.ap
            else:
                kw["scale"] = float(scale)
        writes = [out]
        if accum_out is not None:
            writes.append(accum_out)
            kw["accum_out"] = accum_out.ap
        self.op("act", lambda e: e.activation(out.ap, in_.ap, func, **kw), reads, writes)

    def tt(self, out, in0, in1, op, eng="dve"):
        self.op(eng, lambda e: e.tensor_tensor(out.ap, in0.ap, in1.ap, op), [in0, in1], [out])

    def ts(self, out, in0, s1, op0, s2=None, op1=None, eng="dve"):
        reads = [in0]
        a1 = s1
        if isinstance(s1, V):
            reads.append(s1)
            a1 = s1.ap
        a2 = s2
        if isinstance(s2, V):
            reads.append(s2)
            a2 = s2.ap
        if op1 is None:
            self.op(eng, lambda e: e.tensor_scalar(out.ap, in0.ap, a1, None, op0), reads, [out])
        else:
            self.op(eng, lambda e: e.tensor_scalar(out.ap, in0.ap, a1, a2, op0, op1), reads, [out])

    def stt(self, out, in0, scalar, in1, op0, op1, eng="dve"):
        reads = [in0, in1]
        sc = scalar
        if isinstance(scalar, V):
            reads.append(scalar)
            sc = scalar.ap
        self.op(eng, lambda e: e.scalar_tensor_tensor(out.ap, in0.ap, sc, in1.ap, op0, op1), reads, [out])

    def copy(self, out, in_, eng="dve"):
        if eng == "act":
            self.op("act", lambda e: e.copy(out.ap, in_.ap), [in_], [out])
        else:
            self.op(eng, lambda e: e.tensor_copy(out.ap, in_.ap), [in_], [out])

    def memset(self, out, val, eng="dve"):
        self.op(eng, lambda e: e.memset(out.ap, val), [], [out])

    def generic(self, eng, fn, reads, writes):
        self.op(eng, fn, reads, writes)

    def build(self):
        nc = self.nc
        miles = {e: set() for e in self.ENGS}
        for e in self.ENGS:
            if e != "sp" and self.cnt[e] > 0:
                miles[e].add(self.cnt[e] - 1)
        for e in self.ENGS:
            for (deps, fn, my, dma) in self.ops[e]:
                for (k, v) in deps:
                    if k[0] == "c":
                        miles[k[1]].add(v)
        mnum = {}
        for e in self.ENGS:
            mnum[e] = {idx: n for n, idx in enumerate(sorted(miles[e]))}
            for n in range(len(miles[e])):
                self.semkeys.add(f"c{e}{n // SEM_CH}")

        def ctok(e, idx):
            n = mnum[e][idx]
            return (f"c{e}{n // SEM_CH}", n % SEM_CH + 1)

        fin = [(sk, val) for sk, val in self.last_dma.items()]
        for e in self.ENGS:
            if e != "sp" and self.cnt[e] > 0:
                fin.append(ctok(e, self.cnt[e] - 1))
        self.nwaits = 0
        with ExitStack() as es:
            sem = {k: es.enter_context(nc.semaphore(k)) for k in sorted(self.semkeys)}
            block = es.enter_context(nc.Block())

            def mk(name):
                def body(e):
                    seen = {}
                    for (deps, fn, my, dma) in self.ops[name]:
                        for (k, v) in deps:
                            if seen.get(k, -1) >= v:
                                continue
                            seen[k] = v
                            if k[0] == "c":
                                sk, sv = ctok(k[1], v)
                            else:
                                sk, sv = k[1], v
                            e.wait_ge(sem[sk], sv)
                            self.nwaits += 1
                        ins = fn(e)
                        if dma:
                            ins.then_inc(sem[my[0][1]], 16)
                        elif my[1] in mnum[name]:
                            sk, sv = ctok(name, my[1])
                            ins.then_inc(sem[sk], 1)
                    if name == "sp":
                        for (wk, wv) in fin:
                            e.wait_ge(sem[wk], wv)
                return body

            block.tensor(mk("pe"))
            block.vector(mk("dve"))
            block.scalar(mk("act"))
            block.gpsimd(mk("pool"))
            block.sync(mk("sp"))
        self.es.close()
        return nc

import ml_dtypes
BF = ml_dtypes.bfloat16
TOK = 4352
EPS = 1e-6

def seg_range(s):
    return (0, 256) if s == 0 else (256 + (s - 1) * 512, 512)

def load_cast(P, dst, src, stg, shape1):
    A, B = shape1
    step = max(1, stg[0].shape[1] // B)
    i = 0
    k = getattr(P, "_lc", 0)
    while i < A:
        n = min(step, A - i)
        st = stg[k % len(stg)]
        sv = st[:, 0:n * B].re("p (a b) -> p a b", b=B)
        P.dma(sv, src[:, i:i + n, :])
        P.copy(dst[:, i:i + n, :], sv, eng=("dve" if k % 2 == 0 else "pool"))
        i += n
        k += 1
    P._lc = k

def load_hT(P, hT_d, tok0, n, buf):
    P.dma(buf[:, :, 0:n], hT_d[:, :, tok0:tok0 + n].re("k p t -> p k t"))

def proj(P, out, W, c0, M, hbuf, n):
    for kc in range(16):
        P.mm(out, W[:, kc, c0:c0 + M], hbuf[:, kc, 0:n], start=(kc == 0), stop=(kc == 15))


def fnet(P, hT_d, wf_d, cs_d, tab_d, tabc_d, mix_o):
    m0 = P.mark()
    Wf = P.tile([128, 16, 256], BF16)
    cs = P.tile([128, 256], BF16)
    P.dma(cs.a, cs_d.a)
    AB = [P.tile([128, 34, 256], BF16, nsub=34) for _ in range(2)]
    hb = [P.tile([128, 16, 512], BF16) for _ in range(2)]
    m1 = P.mark()
    stg = [P.tile([128, 4096], F32) for _ in range(2)]
    load_cast(P, Wf.a, wf_d.a, stg, (16, 256))
    P.release(m1)
    UT = [P.tile([128, 512], BF16) for _ in range(2)]
    for s in range(9):
        t0, n = seg_range(s)
        h = hb[s % 2]
        load_hT(P, hT_d, t0, n, h)
        for g in range(2):
            ps = P.psum[g]
            proj(P, ps[:, 0:n], Wf, g * 128, 128, h, n)
            P.copy(UT[g][:, 0:n], ps[:, 0:n], eng="act")
            for tt in range(n // 128):
                pa = P.psum[2 + (tt % 2)]
                P.mm(pa[:, 0:256], UT[g][:, tt * 128:(tt + 1) * 128], cs.a)
                P.copy(AB[g].s(t0 // 128 + tt), pa[:, 0:256], eng=("dve" if tt % 2 == 0 else "act"))
    tb = [P.tile([128, 2, 512], BF16) for _ in range(3)]
    ob = [P.tile([128, 512], BF16) for _ in range(2)]
    k = 0
    for tt in range(2):
        t = tb[k % 3]; k += 1
        P.dma(t[:, :, 0:256], tabc_d[tt])
        for g in range(2):
            P.mm(P.psum[4 + g][:, 0:256], AB[g].s(tt)[:, 0:128], t[:, 0, 0:256], start=(tt == 0), stop=False)
            P.mm(P.psum[4 + g][:, 0:256], AB[g].s(tt)[:, 128:256], t[:, 1, 0:256], start=False, stop=(tt == 1))
    for g in range(2):
        P.copy(ob[g][:, 0:256], P.psum[4 + g][:, 0:256], eng="act")
        P.dma(mix_o[g, :, 0:256], ob[g][:, 0:256], eng="pool")
    for blk in range(8):
        for tt in range(32):
            t = tb[k % 3]; k += 1
            P.dma(t.a, tab_d[blk, tt])
            for g in range(2):
                pz = P.psum[4 + 2 * (blk % 2) + g]
                P.mm(pz.a, AB[g].s(2 + tt)[:, 0:128], t[:, 0, :], start=(tt == 0), stop=False)
                P.mm(pz.a, AB[g].s(2 + tt)[:, 128:256], t[:, 1, :], start=False, stop=(tt == 31))
        for g in range(2):
            pz = P.psum[4 + 2 * (blk % 2) + g]
            P.copy(ob[g].a, pz.a, eng=("act" if g == 0 else "dve"))
            P.dma(mix_o[g, :, 256 + blk * 512:256 + (blk + 1) * 512], ob[g].a, eng="pool")
    P.release(m0)

def fnet_tables():
    T = 4096
    t = np.arange(T, dtype=np.int64)
    m = (t[:, None] * t[None, :]) % T
    ang = 2 * np.pi * m / T
    sc = 1.0 / np.sqrt(T * 128)
    C = (np.cos(ang) * sc).astype(np.float32); S = (-np.sin(ang) * sc).astype(np.float32)
    tab = np.stack([C, S], 0).reshape(2, 32, 128, 8, 512).transpose(3, 1, 2, 0, 4)
    tab = np.ascontiguousarray(tab).astype(BF)
    T2 = 256
    t = np.arange(T2)
    ang = 2 * np.pi * ((t[:, None] * t[None, :]) % T2) / T2
    sc = 1.0 / np.sqrt(T2 * 128)
    Cc = np.cos(ang) * sc; Sc = -np.sin(ang) * sc
    tabc = np.ascontiguousarray(np.stack([Cc, Sc], 0).reshape(2, 2, 128, 256).transpose(1, 2, 0, 3)).astype(BF)
    c = np.arange(128)
    ang = 2 * np.pi * ((c[:, None] * c[None, :]) % 128) / 128
    cs = np.concatenate([np.cos(ang), np.sin(ang)], 1).astype(BF)
    return tab, tabc, cs


CH = 16
import os
STOP = int(os.environ.get('STOP', '99'))

def hgrn_consts():
    identb = np.eye(128, dtype=np.float32).astype(BF)
    s = np.arange(CH)[:, None]; t = np.arange(512)[None, :] % CH
    maskF = (s <= t).astype(np.float32); maskB = (s >= t).astype(np.float32)
    m01 = np.ones((128, 512), np.float32); m01[:, 0::CH] = 0.0
    return identb, np.ascontiguousarray(maskF), np.ascontiguousarray(maskB), m01

def hgrn(P, hT_d, wh_d, lbl_d, msk_d, gn_d, identb_d, maskF_d, maskB_d, m01_d, mix_o):
    m0 = P.mark()
    ident = P.tile([128, 128], BF16); P.dma(ident.a, identb_d.a)
    ones = P.tile([128, 128], BF16); P.memset(ones.a, 1.0)
    epsb = P.tile([128, 1], F32); P.memset(epsb.a, EPS)
    masks = [P.tile([CH, 512], F32), P.tile([CH, 512], F32)]
    P.dma(masks[0].a, maskF_d.a); P.dma(masks[1].a, maskB_d.a)
    m01 = P.tile([128, 512], F32); P.dma(m01.a, m01_d.a)
    gn = P.tile([128, 2], F32); P.dma(gn.a, gn_d.a)
    lbl = P.tile([128, 4, 4], F32); P.dma(lbl.a, lbl_d.a)
    msk = P.tile([128, 4], F32); P.dma(msk.a, msk_d.a)
    el = P.tile([128, 4, 4], F32); P.act(el.a, lbl.a, AF.Exp)
    se = P.tile([128, 4], F32)
    P.op("dve", lambda e: e.reduce_sum(se.ap, el.ap, AX.X), [el.a], [se.a])
    em = P.tile([128, 4, 4], F32)
    P.tt(em.a, el.a, msk.a.re("p (o j) -> p o j", o=1).bc([128, 4, 4]), ALU.mult)
    num = P.tile([128, 4], F32)
    P.op("dve", lambda e: e.reduce_sum(num.ap, em.ap, AX.X), [em.a], [num.a])
    P.op("dve", lambda e: e.reciprocal(se.ap, se.ap), [se.a], [se.a])
    lb = P.tile([128, 4], F32); P.tt(lb.a, num.a, se.a, ALU.mult)
    oml = P.tile([128, 4], F32); P.ts(oml.a, lb.a, -1.0, ALU.mult, 1.0, ALU.add)

    if STOP == 1:
        return
    W = P.tile([128, 16, 640], BF16)
    hb = [P.tile([128, 16, 512], BF16) for _ in range(2)]
    oacc = P.tile([128, TOK], F32)
    sg = P.tile([128, TOK], BF16)
    S = P.tile([128, 128], F32); Sb = P.tile([128, 128], BF16)
    def tset():
        return dict(f=P.tile([128, 512], F32), logf=P.tile([128, 512], F32), k=P.tile([128, 512], F32),
                    a=P.tile([128, 512], F32), a2=P.tile([128, 512], F32), ea=P.tile([128, 512], F32), ena=P.tile([128, 512], F32),
                    ke=P.tile([128, 512], F32), qe=P.tile([128, 512], BF16), keb=P.tile([128, 512], BF16),
                    kdb=P.tile([128, 512], BF16), ib=P.tile([128, 512], BF16),
                    vtok=P.tile([CH, 512 // CH, 128], BF16, nsub=512 // CH), kdtok=P.tile([CH, 512 // CH, 128], BF16, nsub=512 // CH), scm=P.tile([CH, 512], BF16))
    sets = [tset(), tset()]
    m1 = P.mark()
    stg = [P.tile([128, 4096], F32) for _ in range(2)]
    tmp = [P.tile([128, 512], F32) for _ in range(2)]
    ob = [P.tile([128, 512], BF16) for _ in range(2)]

    def prep(T, s, d, hh, h):
        t0, n = seg_range(s)
        ncx = n // CH
        load_hT(P, hT_d, t0, n, h)
        pq, pi, pz, pg = P.psum[0], P.psum[1], P.psum[2], P.psum[3]
        proj(P, pq[:, 0:n], W, 0, 128, h, n)
        proj(P, pi[:, 0:n], W, 128, 128, h, n)
        proj(P, pz[:, 0:n], W, (3 + d) * 128, 128, h, n)
        if d == 1:
            proj(P, pg[:, 0:n], W, 256, 128, h, n)
            P.act(sg[:, t0:t0 + n], pg[:, 0:n], AF.Silu)
        ci = d * 2 + hh
        P.act(T["f"][:, 0:n], pz[:, 0:n], AF.Sigmoid)
        P.ts(T["f"][:, 0:n], T["f"][:, 0:n], oml[:, ci:ci + 1], ALU.mult, lb[:, ci:ci + 1], ALU.add)
        P.act(T["logf"][:, 0:n], T["f"][:, 0:n], AF.Ln)
        P.ts(T["k"][:, 0:n], T["f"][:, 0:n], -1.0, ALU.mult, 1.0, ALU.add, eng="pool")
        P.op("dve", lambda e: e.tensor_tensor_scan(T["a"].ap[:, 0:n], m01.ap[:, 0:n], T["logf"].ap[:, 0:n], 0.0, ALU.mult, ALU.add),
             [m01.a, T["logf"].a], [T["a"].a])
        a = T["a"]
        if d == 1:
            a3 = T["a"][:, 0:n].re("p (c t) -> p c t", t=CH)
            P.tt(T["a2"][:, 0:n], T["logf"][:, 0:n], T["a"][:, 0:n], ALU.subtract)
            P.tt(T["a2"][:, 0:n].re("p (c t) -> p c t", t=CH), T["a2"][:, 0:n].re("p (c t) -> p c t", t=CH),
                 a3[:, :, CH - 1:CH].bc([128, ncx, CH]), ALU.add)
            a = T["a2"]
        P.ts(a[:, 0:n], a[:, 0:n], -80.0, ALU.max)
        P.act(T["ea"][:, 0:n], a[:, 0:n], AF.Exp)
        P.act(T["ena"][:, 0:n], a[:, 0:n], AF.Exp, scale=-1.0)
        P.tt(T["qe"][:, 0:n], pq[:, 0:n], T["ea"][:, 0:n], ALU.mult)
        P.tt(T["ke"][:, 0:n], T["k"][:, 0:n], T["ena"][:, 0:n], ALU.mult)
        P.copy(T["keb"][:, 0:n], T["ke"][:, 0:n], eng="pool")
        ea3 = T["ea"][:, 0:n].re("p (c t) -> p c t", t=CH)
        eend = ea3[:, :, CH - 1:CH] if d == 0 else ea3[:, :, 0:1]
        P.tt(T["kdb"][:, 0:n].re("p (c t) -> p c t", t=CH), T["ke"][:, 0:n].re("p (c t) -> p c t", t=CH),
             eend.bc([128, ncx, CH]), ALU.mult)
        P.copy(T["ib"][:, 0:n], pi[:, 0:n], eng="act")
        pt = P.psum[4]
        for c0 in range(0, ncx, 4):
            for c in range(c0, c0 + 4):
                P.mm(pt[0:CH, (c - c0) * 128:(c - c0 + 1) * 128], T["ib"][:, c * CH:(c + 1) * CH], ident.a)
            P.copy(T["vtok"].s(c0, c0 + 4), pt[0:CH, :].re("p (a b) -> p a b", b=128), eng="act")
            for c in range(c0, c0 + 4):
                P.mm(pt[0:CH, (c - c0) * 128:(c - c0 + 1) * 128], T["kdb"][:, c * CH:(c + 1) * CH], ident.a)
            P.copy(T["kdtok"].s(c0, c0 + 4), pt[0:CH, :].re("p (a b) -> p a b", b=128), eng="dve")
        psc = P.psum[5]
        for c in range(ncx):
            P.mm(psc[0:CH, c * CH:(c + 1) * CH], T["keb"][:, c * CH:(c + 1) * CH], T["qe"][:, c * CH:(c + 1) * CH])
        P.tt(T["scm"][:, 0:n], psc[0:CH, 0:n], masks[d][:, 0:n], ALU.mult)

    def loop(T, s, d):
        t0, n = seg_range(s)
        ncx = n // CH
        pO = P.psum[6]; pSs = [P.psum[7], P.psum[3]]
        order = range(ncx) if d == 0 else range(ncx - 1, -1, -1)
        for ii, c in enumerate(order):
            cs = slice(c * CH, (c + 1) * CH)
            P.mm(pO[:, cs], T["vtok"].s(c), T["scm"][:, cs], start=True, stop=False)
            pS = pSs[ii % 2]
            P.mm(pS[:, 0:128], T["kdtok"].s(c), T["vtok"].s(c))
            P.mm(pO[:, cs], Sb.a, T["qe"][:, cs], start=False, stop=True)
            col = c * CH + CH - 1 if d == 0 else c * CH
            P.stt(S.a, S.a, T["ea"][:, col:col + 1], pS[:, 0:128], ALU.mult, ALU.add)
            P.copy(Sb.a, S.a, eng="act")
        if d == 0:
            P.copy(oacc[:, t0:t0 + n], pO[:, 0:n], eng="act")
        else:
            P.tt(oacc[:, t0:t0 + n], oacc[:, t0:t0 + n], pO[:, 0:n], ALU.add)

    for hh in range(2):
        load_cast(P, W.a.re("p k (j c) -> p k j c", j=5), wh_d[:, :, :, hh * 128:(hh + 1) * 128], stg, (16, 640)) if False else None
        for j in range(5):
            for half in range(2):
                st = stg[(j * 2 + half) % 2]
                sv = st[:, 0:1024].re("p (a b) -> p a b", b=128)
                P.dma(sv, wh_d[:, half * 8:(half + 1) * 8, j, hh * 128:(hh + 1) * 128])
                P.copy(W[:, half * 8:(half + 1) * 8, j * 128:(j + 1) * 128], sv, eng=("dve" if half == 0 else "pool"))
        for d in range(2):
            P.memset(S.a, 0.0); P.memset(Sb.a, 0.0)
            segs = list(range(9)) if d == 0 else [0] + list(range(8, 0, -1))
            prep(sets[0], segs[0], d, hh, hb[0])
            if STOP == 2:
                return
            for i, s in enumerate(segs):
                if i + 1 < len(segs):
                    prep(sets[(i + 1) % 2], segs[i + 1], d, hh, hb[(i + 1) % 2])
                loop(sets[i % 2], s, d)
                if STOP == 3:
                    return
        for s in range(9):
            t0, n = seg_range(s)
            sq = sets[0]["qe"]
            P.act(sq[:, 0:n], oacc[:, t0:t0 + n], AF.Square)
            pss = P.psum[s % 2]
            P.mm(pss[:, 0:n], ones.a, sq[:, 0:n])
            r = tmp[s % 2]
            P.act(r[:, 0:n], pss[:, 0:n], AF.Sqrt, bias=epsb.a, scale=1.0 / 128)
            P.op("dve", lambda e, r=r, n=n: e.reciprocal(r.ap[:, 0:n], r.ap[:, 0:n]), [r.a], [r.a])
            P.tt(r[:, 0:n], r[:, 0:n], oacc[:, t0:t0 + n], ALU.mult)
            P.tt(r[:, 0:n], r[:, 0:n], sg[:, t0:t0 + n], ALU.mult, eng="pool")
            o = ob[s % 2]
            P.ts(o[:, 0:n], r[:, 0:n], gn[:, hh:hh + 1], ALU.mult)
            P.dma(mix_o[hh, :, t0:t0 + n], o[:, 0:n], eng="pool")
    P.release(m0)

def hgrn_weights(d, l, hf):
    w_in = d['w_in'][l]
    wh = w_in[:, 512:512 + 5 * 512].reshape(2048, 5, 4, 128)[:, :, hf * 2:(hf + 1) * 2].reshape(2048, 5, 256)
    wh = np.ascontiguousarray(wh.reshape(16, 128, 5, 256).transpose(1, 0, 2, 3))
    lg = d['hgrn_lb_logits']
    lbl = lg.reshape(4, 2, 4, 128)[:, :, hf * 2:(hf + 1) * 2]
    lbl = np.ascontiguousarray(lbl.transpose(3, 1, 2, 0).reshape(128, 4, 4))
    msk = np.zeros((128, 4), np.float32); msk[:, 1:l + 1] = 1.0
    gn = np.ascontiguousarray(d['hgrn_norm'][l].reshape(4, 128)[hf * 2:(hf + 1) * 2].T)
    return dict(wh=wh, lbl=lbl, msk=msk, gn=gn)

def hgrn_decl(P):
    return [P.dram("wh", [128, 16, 5, 256], F32, "ExternalInput"), P.dram("lbl", [128, 4, 4], F32, "ExternalInput"),
            P.dram("msk", [128, 4], F32, "ExternalInput"), P.dram("gn", [128, 2], F32, "ExternalInput"),
            P.dram("identb", [128, 128], BF16, "ExternalInput"), P.dram("maskF", [CH, 512], F32, "ExternalInput"),
            P.dram("maskB", [CH, 512], F32, "ExternalInput"), P.dram("m01", [128, 512], F32, "ExternalInput")]


import os
STOP = int(os.environ.get('STOP', '99'))
MLA_SCALE = 192 ** -0.5

def rmsnorm_fm(P, ps_list, n, nch, dim, g, out, seg0, sq, ones, pss, rstd, epsb):
    for j in range(nch):
        P.act(sq[:, j, 0:n], ps_list[j], AF.Square)
    for j in range(nch):
        P.mm(pss[:, 0:n], ones.a, sq[:, j, 0:n], start=(j == 0), stop=(j == nch - 1))
    P.act(rstd[:, 0:n], pss[:, 0:n], AF.Sqrt, bias=epsb.a, scale=1.0 / dim)
    P.op("dve", lambda e: e.reciprocal(rstd.ap[:, 0:n], rstd.ap[:, 0:n]), [rstd.a], [rstd.a])
    for j in range(nch):
        P.stt(out[:, j, seg0:seg0 + n], ps_list[j], g[:, j:j + 1], rstd[:, 0:n], ALU.mult, ALU.mult)

def rope_fm(P, out, a_ps, b_ps, cos, sin, n, t1, t2):
    P.tt(t1[:, 0:n], a_ps, cos[:, 0:n], ALU.mult)
    P.tt(t2[:, 0:n], b_ps, sin[:, 0:n], ALU.mult)
    P.tt(out, t1[:, 0:n], t2[:, 0:n], ALU.add, eng="pool")

def mla(P, hT_d, wq_d, wkv_d, wkr_d, wuq_d, wuk_d, wuv_d, gq_d, gkv_d, cos_d, sin_d, mix_o):
    m0 = P.mark()
    ones = P.tile([128, 128], BF16); P.memset(ones.a, 1.0)
    epsb = P.tile([128, 1], F32); P.memset(epsb.a, EPS)
    gq = P.tile([128, 4], F32); P.dma(gq.a, gq_d.a)
    gkv = P.tile([128, 2], F32); P.dma(gkv.a, gkv_d.a)
    cqn = P.tile([128, 4, TOK], BF16)
    ckvn = P.tile([128, 2, TOK], BF16)
    KR = P.tile([64, TOK], BF16)
    Wuq = P.tile([128, 4, 1024], BF16)
    Wuk = P.tile([128, 2, 512], BF16)
    Wuv = P.tile([128, 2, 512], BF16)
    cosb = [P.tile([64, 512], F32) for _ in range(2)]
    sinb = [P.tile([64, 512], F32) for _ in range(2)]
    t1 = P.tile([64, 512], F32); t2 = P.tile([64, 512], F32)
    rstd = P.tile([128, 512], F32)
    m1 = P.mark()
    Wq = P.tile([128, 16, 512], BF16)
    Wkv = P.tile([128, 16, 256], BF16)
    Wkr = P.tile([128, 16, 128], BF16)
    hb = [P.tile([128, 16, 512], BF16) for _ in range(2)]
    sq = P.tile([128, 4, 512], BF16)
    stg = [P.tile([128, 4096], F32) for _ in range(2)]
    load_cast(P, Wq.a, wq_d.a, stg, (16, 512))
    load_cast(P, Wkv.a, wkv_d.a, stg, (16, 256))
    load_cast(P, Wkr.a, wkr_d.a, stg, (16, 128))
    load_cast(P, Wuq.a, wuq_d.a, stg, (4, 1024))
    load_cast(P, Wuk.a, wuk_d.a, stg, (2, 512))
    load_cast(P, Wuv.a, wuv_d.a, stg, (2, 512))
    for s in range(9):
        t0, n = seg_range(s)
        h = hb[s % 2]
        load_hT(P, hT_d, t0, n, h)
        if s > 0:
            P.dma(cosb[s % 2].a, cos_d[:, (s - 1) * 512:s * 512])
            P.dma(sinb[s % 2].a, sin_d[:, (s - 1) * 512:s * 512])
        for j in range(4):
            proj(P, P.psum[j][:, 0:n], Wq, j * 128, 128, h, n)
        rmsnorm_fm(P, [P.psum[j][:, 0:n] for j in range(4)], n, 4, 512, gq, cqn, t0, sq, ones, P.psum[4], rstd, epsb)
        for j in range(2):
            proj(P, P.psum[5 + j][:, 0:n], Wkv, j * 128, 128, h, n)
        rmsnorm_fm(P, [P.psum[5 + j][:, 0:n] for j in range(2)], n, 2, 256, gkv, ckvn, t0, sq, ones, P.psum[7], rstd, epsb)
        proj(P, P.psum[0][0:64, 0:n], Wkr, 0, 64, h, n)
        if s == 0:
            P.copy(KR[:, t0:t0 + n], P.psum[0][0:64, 0:n], eng="act")
        else:
            proj(P, P.psum[1][0:64, 0:n], Wkr, 64, 64, h, n)
            rope_fm(P, KR[:, t0:t0 + n], P.psum[0][0:64, 0:n], P.psum[1][0:64, 0:n], cosb[s % 2], sinb[s % 2], n, t1, t2)
    P.release(m1)
    if STOP == 1:
        return
    QN = P.tile([128, TOK], BF16); QR = P.tile([64, TOK], BF16); KN = P.tile([128, TOK], BF16)
    Vh = P.tile([128, 34, 128], BF16)
    PT = [P.tile([128, 512], BF16) for _ in range(3)]
    rden = P.tile([128, 512], F32)
    ob = [P.tile([128, 512], BF16) for _ in range(2)]
    for hd in range(4):
        for s in range(9):
            t0, n = seg_range(s)
            if s > 0:
                P.dma(cosb[s % 2].a, cos_d[:, (s - 1) * 512:s * 512])
                P.dma(sinb[s % 2].a, sin_d[:, (s - 1) * 512:s * 512])
            ps = P.psum[0]
            for cc in range(4):
                P.mm(ps[:, 0:n], Wuq[:, cc, hd * 256:hd * 256 + 128], cqn[:, cc, t0:t0 + n], start=(cc == 0), stop=(cc == 3))
            P.copy(QN[:, t0:t0 + n], ps[:, 0:n], eng="act")
            ps1 = P.psum[1]; ps2 = P.psum[2]
            for cc in range(4):
                P.mm(ps1[0:64, 0:n], Wuq[:, cc, hd * 256 + 128:hd * 256 + 192], cqn[:, cc, t0:t0 + n], start=(cc == 0), stop=(cc == 3))
            if s == 0:
                P.copy(QR[:, t0:t0 + n], ps1[0:64, 0:n], eng="act")
            else:
                for cc in range(4):
                    P.mm(ps2[0:64, 0:n], Wuq[:, cc, hd * 256 + 192:hd * 256 + 256], cqn[:, cc, t0:t0 + n], start=(cc == 0), stop=(cc == 3))
                rope_fm(P, QR[:, t0:t0 + n], ps1[0:64, 0:n], ps2[0:64, 0:n], cosb[s % 2], sinb[s % 2], n, t1, t2)
            ps3 = P.psum[3]
            for cc in range(2):
                P.mm(ps3[:, 0:n], Wuk[:, cc, hd * 128:(hd + 1) * 128], ckvn[:, cc, t0:t0 + n], start=(cc == 0), stop=(cc == 1))
            P.copy(KN[:, t0:t0 + n], ps3[:, 0:n], eng="dve")
            ps4 = P.psum[4]
            for tt in range(n // 128):
                for cc in range(2):
                    P.mm(ps4[:, tt * 128:(tt + 1) * 128], ckvn[:, cc, t0 + tt * 128:t0 + (tt + 1) * 128],
                         Wuv[:, cc, hd * 128:(hd + 1) * 128], start=(cc == 0), stop=(cc == 1))
            P.copy(Vh[:, t0 // 128:t0 // 128 + n // 128, :], ps4[:, 0:n].re("p (a b) -> p a b", b=128), eng="act")
        if STOP == 2:
            return
        k = 0
        for qb in range(9):
            q0, nq = seg_range(qb)
            nkt = 2 if qb == 0 else 34
            pO = P.psum[5]; pD = P.psum[6]
            for kt in range(nkt):
                pS = P.psum[kt % 2 + (0 if qb % 2 == 0 else 2)]
                P.mm(pS[:, 0:nq], KN[:, kt * 128:(kt + 1) * 128], QN[:, q0:q0 + nq], start=True, stop=False)
                P.mm(pS[:, 0:nq], KR[:, kt * 128:(kt + 1) * 128], QR[:, q0:q0 + nq], start=False, stop=True)
                pt = PT[k % 3]; k += 1
                P.act(pt[:, 0:nq], pS[:, 0:nq], AF.Exp, scale=MLA_SCALE)
                P.mm(pO[:, 0:nq], Vh[:, kt, :], pt[:, 0:nq], start=(kt == 0), stop=(kt == nkt - 1))
                P.mm(pD[:, 0:nq], ones.a, pt[:, 0:nq], start=(kt == 0), stop=(kt == nkt - 1))
            P.op("dve", lambda e, nq=nq: e.reciprocal(rden.ap[:, 0:nq], pD.ap[:, 0:nq]), [pD.a], [rden.a])
            o = ob[qb % 2]
            P.tt(o[:, 0:nq], pO[:, 0:nq], rden[:, 0:nq], ALU.mult)
            P.dma(mix_o[hd, :, q0:q0 + nq], o[:, 0:nq], eng="pool")
            if STOP == 3:
                return
    P.release(m0)

def rope_tables():
    t = np.arange(4096)
    row, col = t // 64, t % 64
    inv = 10000.0 ** (-np.arange(16, dtype=np.float32) / 16)
    ang = np.concatenate([row[:, None] * inv, col[:, None] * inv], -1)
    cos, sin = np.cos(ang).T, np.sin(ang).T
    COS2 = np.concatenate([cos, cos], 0).astype(np.float32)
    SIN2 = np.concatenate([-sin, sin], 0).astype(np.float32)
    return np.ascontiguousarray(COS2), np.ascontiguousarray(SIN2)

PERM = np.concatenate([np.arange(0, 64, 2), np.arange(1, 64, 2)])
PERM_SW = np.concatenate([np.arange(1, 64, 2), np.arange(0, 64, 2)])

def mla_weights(d, l, hf):
    w_in = d['w_in'][l]
    o = 512 + 5 * 512
    wq = w_in[:, o:o + 512]; wkv = w_in[:, o + 512:o + 768]; wkr = w_in[:, o + 768:o + 832]
    wkr2 = np.concatenate([wkr[:, PERM], wkr[:, PERM_SW]], 1)
    fm = lambda w: np.ascontiguousarray(w.reshape(w.shape[0] // 128, 128, -1).transpose(1, 0, 2))
    wuq = d['w_uq'][l].reshape(512, 8, 192)[:, hf * 4:(hf + 1) * 4]
    wuq2 = np.concatenate([wuq[:, :, :128], wuq[:, :, 128:][:, :, PERM], wuq[:, :, 128:][:, :, PERM_SW]], -1).reshape(512, 1024)
    wukv = d['w_ukv'][l].reshape(256, 8, 256)[:, hf * 4:(hf + 1) * 4]
    wuk = wukv[:, :, :128].reshape(256, 512); wuv = wukv[:, :, 128:].reshape(256, 512)
    return dict(wq=fm(wq), wkv=fm(wkv), wkr=fm(wkr2), wuq=fm(wuq2), wuk=fm(wuk), wuv=fm(wuv),
                gq=np.ascontiguousarray(d['mla_q_norm'][l].reshape(4, 128).T), gkv=np.ascontiguousarray(d['mla_kv_norm'][l].reshape(2, 128).T))

def mla_decl(P):
    return [P.dram("wq", [128, 16, 512], F32, "ExternalInput"), P.dram("wkv", [128, 16, 256], F32, "ExternalInput"),
            P.dram("wkr", [128, 16, 128], F32, "ExternalInput"), P.dram("wuq", [128, 4, 1024], F32, "ExternalInput"),
            P.dram("wuk", [128, 2, 512], F32, "ExternalInput"), P.dram("wuv", [128, 2, 512], F32, "ExternalInput"),
            P.dram("gq", [128, 4], F32, "ExternalInput"), P.dram("gkv", [128, 2], F32, "ExternalInput"),
            P.dram("cos2", [64, 4096], F32, "ExternalInput"), P.dram("sin2", [64, 4096], F32, "ExternalInput")]


NT = 17
ALPHA = 8 ** 0.25
GROUPS = [(0, 6), (6, 12), (12, 17)]

def ln_tile(P, xt, xn, st, mv, rstd, epsb):
    for c in range(4):
        P.op("dve", lambda e, c=c: e.bn_stats(st.ap[:, c, :], xt.ap[:, c * 512:(c + 1) * 512]), [xt], [st.a])
    P.op("dve", lambda e: e.bn_aggr(mv.ap, st.ap.rearrange("p a b -> p (a b)")), [st.a], [mv.a])
    P.act(rstd.a, mv[:, 1:2], AF.Sqrt, bias=epsb.a)
    P.op("dve", lambda e: e.reciprocal(rstd.ap, rstd.ap), [rstd.a], [rstd.a])
    P.ts(xn, xt, mv[:, 0:1], ALU.subtract, rstd.a, ALU.mult)

def phase_c(P, x_d, mix_d, wout_d, modT_d, bc1_d, bc2_d, wr_d, rbb_d, w1_d, b1g_d, b1l_d, w2_d, b2_d, identf_d,
            x1_d, h2_d, out_d):
    epsb = P.tile([128, 1], F32); P.memset(epsb.a, EPS)
    identf = P.tile([128, 128], F32); P.dma(identf.a, identf_d.a)
    modT = P.tile([128, 96, 2], F32); P.dma(modT.a, modT_d.a)
    sc2p = P.tile([128, 16, 2], F32); P.ts(sc2p.a, modT[:, 64:80, :], 1.0, ALU.add)
    G = P.tile([128, NT, 32], F32)
    GT = P.tile([32, NT * 128], F32)
    st = P.tile([128, 4, 6], F32); mv = P.tile([128, 2], F32); rstd = P.tile([128, 1], F32)
    b1g = P.tile([128, 32, 6], F32); P.dma(b1g.a, b1g_d.a)
    b1l = P.tile([128, 32, 6], F32); P.dma(b1l.a, b1l_d.a)
    m0 = P.mark()
    Wout = P.tile([128, 16, 2048], BF16)
    bc = [P.tile([128, 2048], F32) for _ in range(4)]
    for i in range(4):
        P.dma(bc[i].a, V(bc1_d.ap[i].partition_broadcast(128), bc1_d.subs))
    Wr = P.tile([128, 16, 32], F32); P.dma(Wr.a, wr_d.a)
    rbb = P.tile([128, 32], F32); P.dma(rbb.a, rbb_d.a)
    mx = [P.tile([128, 16, 128], BF16) for _ in range(2)]
    xt = [P.tile([128, 2048], F32) for _ in range(2)]
    yt = P.tile([128, 2048], F32)
    x1 = [P.tile([128, 2048], F32) for _ in range(2)]
    h2b = [P.tile([128, 16, 128], BF16) for _ in range(2)]
    h2f = P.tile([128, 16, 128], F32)
    lg = P.tile([128, 32], F32); m8 = P.tile([128, 8], F32); negm = P.tile([128, 1], F32)
    mk = P.tile([128, 32], F32); ex = P.tile([128, 32], F32); ssum = P.tile([128, 1], F32)
    m1 = P.mark()
    stg = [P.tile([128, 2048], F32) for _ in range(2)]
    load_cast(P, Wout.a, wout_d.a, stg, (16, 2048))
    P.release(m1)
    for t in range(NT):
        n = 1 if t == 0 else 0
        ts_ = slice(t * 128, (t + 1) * 128)
        P.dma(mx[t % 2].a, mix_d[:, :, ts_].re("k p t -> p k t"))
        P.dma(xt[t % 2].a, x_d[ts_, :])
        for db in range(4):
            for fc in range(16):
                P.mm(P.psum[db].a, mx[t % 2][:, fc, :], Wout[:, fc, db * 512:(db + 1) * 512], start=(fc == 0), stop=(fc == 15))
            P.tt(yt[:, db * 512:(db + 1) * 512], P.psum[db].a, bc[n][:, db * 512:(db + 1) * 512], ALU.mult)
        P.stt(yt.a, xt[t % 2].a, ALPHA, yt.a, ALU.mult, ALU.add)
        X1 = x1[t % 2]
        ln_tile(P, yt.a, X1.a, st, mv, rstd, epsb)
        P.tt(X1.a, X1.a, bc[2].a, ALU.mult, eng="pool")
        P.tt(X1.a, X1.a, bc[3].a, ALU.add, eng="pool")
        P.dma(x1_d[ts_, :], X1.a, eng="pool")
        ln_tile(P, X1.a, yt.a, st, mv, rstd, epsb)
        hb_ = h2b[t % 2]
        for kc in range(16):
            pb = P.psum[4 + (kc // 4) % 4]
            pv = pb[:, (kc % 4) * 128:(kc % 4 + 1) * 128]
            P.transpose(pv, yt[:, kc * 128:(kc + 1) * 128], identf.a)
            if kc % 4 == 3:
                g4 = kc // 4
                for q in range(4):
                    k2 = g4 * 4 + q
                    P.act(h2f[:, k2, :], pb[:, q * 128:(q + 1) * 128], AF.Identity,
                          bias=modT[:, 48 + k2, n:n + 1], scale=sc2p[:, k2, n:n + 1])
                P.copy(hb_[:, g4 * 4:(g4 + 1) * 4, :], h2f[:, g4 * 4:(g4 + 1) * 4, :], eng="dve")
        P.dma(h2_d[:, :, ts_].re("k p t -> p k t"), hb_.a, eng="pool")
        pr = P.psum[0]
        for kc in range(16):
            P.mm(pr[:, 0:32], h2f[:, kc, :], Wr[:, kc, :], start=(kc == 0), stop=(kc == 15))
        P.tt(lg.a, pr[:, 0:32], rbb.a, ALU.add)
        P.op("dve", lambda e: e.max(m8.ap, lg.ap), [lg.a], [m8.a])
        P.ts(negm.a, m8[:, 0:1], -1.0, ALU.mult)
        P.ts(mk.a, lg.a, m8[:, 3:4], ALU.is_ge)
        P.act(ex.a, lg.a, AF.Exp, bias=negm.a)
        P.tt(ex.a, ex.a, mk.a, ALU.mult)
        P.op("dve", lambda e: e.reduce_sum(ssum.ap, ex.ap, AX.X), [ex.a], [ssum.a])
        P.op("dve", lambda e: e.reciprocal(ssum.ap, ssum.ap), [ssum.a], [ssum.a])
        P.ts(G[:, t, :], ex.a, ssum.a, ALU.mult)
        pg = P.psum[1]
        P.transpose(pg[0:32, 0:128], G[:, t, :], identf.a)
        P.copy(GT[:, ts_], pg[0:32, 0:128], eng="act")
    P.release(m0)
    bcg = P.tile([128, 2048], F32)
    bc2 = [bcg, bcg, P.tile([128, 2048], F32), P.tile([128, 2048], F32)]
    P.dma(bcg.a, V(bc2_d.ap[1].partition_broadcast(128), bc2_d.subs))
    P.dma(bc2[2].a, V(bc2_d.ap[2].partition_broadcast(128), bc2_d.subs))
    P.dma(bc2[3].a, V(bc2_d.ap[3].partition_broadcast(128), bc2_d.subs))
    b2s = P.tile([32, 2048], F32); P.dma(b2s.a, b2_d.a)
    H2g = P.tile([128, 16, 768], BF16)
    acc = P.tile([128, 6, 2048], F32, nsub=6)
    actT = P.tile([128, 6, 768], BF16, nsub=6)
    W1p = [P.tile([128, 16, 256], BF16) for _ in range(2)]
    W2 = P.tile([128, 6, 2048], BF16, nsub=6)
    stg = [P.tile([128, 2048], F32) for _ in range(2)]
    tmps = [dict(xg=P.tile([128, 512], F32), sg=P.tile([128, 512], F32), xl=P.tile([128, 512], F32)) for _ in range(2)]
    xe, ye = stg
    kk = [0]
    def load_w1(e, fc, buf):
        for half in range(2):
            s_ = stg[kk[0] % 2]; kk[0] += 1
            sv = s_.a.re("p (a b) -> p a b", b=256)
            P.dma(sv, w1_d[e, fc, :, half * 8:(half + 1) * 8, :])
            P.copy(buf[:, half * 8:(half + 1) * 8, :], sv, eng=("dve" if half == 0 else "pool"))
    def load_w2(e):
        for fc in range(6):
            s_ = stg[kk[0] % 2]; kk[0] += 1
            P.dma(s_.a, w2_d[e, :, fc, :])
            P.copy(W2.s(fc), s_.a, eng=("pool" if fc % 2 == 0 else "dve"))
    for (ta, tb) in GROUPS:
        ng = tb - ta
        ntok = ng * 128
        P.dma(H2g[:, :, 0:ntok], h2_d[:, :, ta * 128:tb * 128].re("k p t -> p k t"))
        for ti in range(ng):
            for db in range(4):
                pb = P.psum[4 + db]
                P.mm(pb.a, GT[:, (ta + ti) * 128:(ta + ti + 1) * 128], b2s[:, db * 512:(db + 1) * 512])
                P.copy(acc.s(ti)[:, db * 512:(db + 1) * 512], pb.a, eng=("act" if db % 2 == 0 else "dve"))
        blocks = [(c0, min(c0 + 512, ntok)) for c0 in range(0, ntok, 512)]
        pieces = [(e, fc) for e in range(32) for fc in range(6)]
        load_w1(0, 0, W1p[0])
        for pi_, (e, fc) in enumerate(pieces):
            if pi_ + 1 < len(pieces):
                load_w1(*pieces[pi_ + 1], W1p[(pi_ + 1) % 2])
            if fc == 0:
                load_w2(e)
            Wp = W1p[pi_ % 2]
            for bi, (c0, c1) in enumerate(blocks):
                T = tmps[bi % 2]
                n = c1 - c0
                pg_, pl_ = P.psum[2 * (bi % 2)], P.psum[2 * (bi % 2) + 1]
                for kc in range(16):
                    P.mm(pg_[:, 0:n], Wp[:, kc, 0:128], H2g[:, kc, c0:c1], start=(kc == 0), stop=(kc == 15))
                for kc in range(16):
                    P.mm(pl_[:, 0:n], Wp[:, kc, 128:256], H2g[:, kc, c0:c1], start=(kc == 0), stop=(kc == 15))
                P.ts(T["xg"][:, 0:n], pg_[:, 0:n], b1g[:, e, fc:fc + 1], ALU.add, 7.0, ALU.min)
                P.act(T["sg"][:, 0:n], T["xg"][:, 0:n], AF.Sigmoid, scale=1.702)
                P.ts(T["xl"][:, 0:n], pl_[:, 0:n], b1l[:, e, fc:fc + 1], ALU.add, 7.0, ALU.min)
                P.ts(T["xl"][:, 0:n], T["xl"][:, 0:n], -7.0, ALU.max, 1.0, ALU.add, eng="pool")
                P.tt(T["xg"][:, 0:n], T["xg"][:, 0:n], T["sg"][:, 0:n], ALU.mult, eng="pool")
                P.tt(actT.s(fc)[:, c0:c1], T["xg"][:, 0:n], T["xl"][:, 0:n], ALU.mult)
            if fc == 5:
                for ti in range(ng):
                    for db in range(4):
                        pb = P.psum[4 + db]
                        for f2 in range(6):
                            P.mm(pb.a, actT.s(f2)[:, ti * 128:(ti + 1) * 128], W2.s(f2)[:, db * 512:(db + 1) * 512],
                                 start=(f2 == 0), stop=(f2 == 5))
                        av = acc.s(ti)[:, db * 512:(db + 1) * 512]
                        P.stt(av, pb.a, G[:, ta + ti, e:e + 1], av, ALU.mult, ALU.add)
        for ti in range(ng):
            t = ta + ti
            n = 1 if t == 0 else 0
            ts_ = slice(t * 128, (t + 1) * 128)
            P.dma(xe.a, x1_d[ts_, :])
            P.tt(ye.a, acc.s(ti), bc2[n].a, ALU.mult)
            P.stt(ye.a, xe.a, ALPHA, ye.a, ALU.mult, ALU.add)
            ln_tile(P, ye.a, xe.a, st, mv, rstd, epsb)
            P.tt(xe.a, xe.a, bc2[2].a, ALU.mult, eng="pool")
            P.tt(xe.a, xe.a, bc2[3].a, ALU.add, eng="pool")
            P.dma(out_d[ts_, :], xe.a, eng="pool")
            if t == 0:
                P.dma(bcg.a, V(bc2_d.ap[0].partition_broadcast(128), bc2_d.subs))

def c_decl(P):
    I = "ExternalInput"
    return [P.dram("x", [NT * 128, 2048], F32, I), P.dram("mix", [16, 128, NT * 128], BF16, I),
            P.dram("wout", [128, 16, 2048], F32, I), P.dram("modT", [128, 96, 2], F32, I),
            P.dram("bc1", [4, 2048], F32, I), P.dram("bc2", [4, 2048], F32, I),
            P.dram("wr", [128, 16, 32], F32, I), P.dram("rbb", [128, 32], F32, I),
            P.dram("w1r", [32, 6, 128, 16, 256], F32, I), P.dram("b1g", [128, 32, 6], F32, I), P.dram("b1l", [128, 32, 6], F32, I),
            P.dram("w2r", [32, 128, 6, 2048], F32, I), P.dram("b2", [32, 2048], F32, I), P.dram("identf", [128, 128], F32, I),
            P.dram("x1s", [NT * 128, 2048], F32, "Internal"), P.dram("h2s", [16, 128, NT * 128], BF16, "Internal"),
            P.dram("out", [NT * 128, 2048], F32, "ExternalOutput")]

def c_weights(d, l):
    fm = lambda w: np.ascontiguousarray(w.reshape(w.shape[0] // 128, 128, -1).transpose(1, 0, 2))
    w1 = d['w1'][l]
    w1g = w1[:, :, 0::2].reshape(32, 16, 128, 6, 128); w1l = w1[:, :, 1::2].reshape(32, 16, 128, 6, 128)
    w1r = np.ascontiguousarray(np.concatenate([w1g, w1l], -1).transpose(0, 3, 2, 1, 4))
    b1 = d['b1'][l]
    b1g = np.ascontiguousarray(b1[:, 0::2].reshape(32, 6, 128).transpose(2, 0, 1))
    b1l = np.ascontiguousarray(b1[:, 1::2].reshape(32, 6, 128).transpose(2, 0, 1))
    w2r = np.ascontiguousarray(d['w2'][l].reshape(32, 6, 128, 2048).transpose(0, 2, 1, 3))
    return dict(wout=fm(d['w_out'][l]), wr=fm(d['router_w'][l]), rbb=np.ascontiguousarray(np.broadcast_to(d['router_b'][l], (128, 32))),
                w1r=w1r, b1g=b1g, b1l=b1l, w2r=w2r, b2=np.ascontiguousarray(d['b2'][l]), identf=np.eye(128, dtype=np.float32))

def c_bcasts(d, l, modT):
    def vec(j0, n):
        return np.ascontiguousarray(modT[:, j0:j0 + 16, n].T.reshape(2048))
    B = lambda v: np.ascontiguousarray(v)
    bc1 = np.stack([vec(32, 0), vec(32, 1), B(d['ln1_g'][l]), B(d['ln1_b'][l])])
    bc2 = np.stack([vec(80, 0), vec(80, 1), B(d['ln2_g'][l]), B(d['ln2_b'][l])])
    return bc1, bc2


import ml_dtypes


def ln_tile_a(P, xt, xn, st, mv, rstd, epsb):
    for c in range(4):
        P.op("dve", lambda e, c=c: e.bn_stats(st.ap[:, c, :], xt.ap[:, c * 512:(c + 1) * 512]), [xt], [st.a])
    P.op("dve", lambda e: e.bn_aggr(mv.ap, st.ap.rearrange("p a b -> p (a b)")), [st.a], [mv.a])
    P.act(rstd.a, mv[:, 1:2], AF.Sqrt, bias=epsb.a)
    P.op("dve", lambda e: e.reciprocal(rstd.ap, rstd.ap), [rstd.a], [rstd.a])
    P.ts(xn, xt, mv[:, 0:1], ALU.subtract, rstd.a, ALU.mult)

def build_a():
    nc = bass.Bass("TRN2", target_bir_lowering=False)
    P = Prog(nc)
    x = P.dram("x", [NT * 128, 2048], F32, "ExternalInput")
    cT = P.dram("cT", [128, 16, 2], F32, "ExternalInput")
    wada = P.dram("w_ada", [2048, 12288], F32, "ExternalInput")
    bT = P.dram("bT", [128, 96], F32, "ExternalInput")
    ident_d = P.dram("ident", [128, 128], F32, "ExternalInput")
    hT_o = P.dram("hT", [16, 128, NT * 128], BF16, "ExternalOutput")
    mod_o = P.dram("modT", [128, 96, 2], F32, "ExternalOutput")

    ident = P.tile([128, 128], F32)
    P.dma(ident.a, ident_d.a)
    ct = P.tile([128, 16, 2], F32)
    P.dma(ct.a, cT.a)
    sil = P.tile([128, 16, 2], F32)
    P.act(sil.a, ct.a, AF.Silu)
    bt = P.tile([128, 96], F32)
    P.dma(bt.a, bT.a)
    modT = P.tile([128, 96, 2], F32)
    m = P.mark()
    panels = [P.tile([128, 12288], F32) for _ in range(2)]
    ps = P.psum[0]
    for kc in range(16):
        pn = panels[kc % 2]
        P.dma(pn.a, wada[kc * 128:(kc + 1) * 128, :])
        for j in range(96):
            P.mm(ps[:, 2 * j:2 * j + 2], pn[:, j * 128:(j + 1) * 128], sil[:, kc, :],
                 start=(kc == 0 and j == 0), stop=(kc == 15 and j == 95))
    P.tt(modT.a, ps[:, 0:192].re("p (j n) -> p j n", n=2), bt.a.re("p (j o) -> p j o", o=1).bc([128, 96, 2]), ALU.add)
    P.release(m)
    P.dma(mod_o.a, modT.a)
    sc1p = P.tile([128, 16, 2], F32)
    P.ts(sc1p.a, modT[:, 16:32, :], 1.0, ALU.add)
    xts = [P.tile([128, 2048], F32) for _ in range(2)]
    xns = [P.tile([128, 2048], F32) for _ in range(2)]
    hts = [P.tile([128, 16, 128], BF16) for _ in range(2)]
    st = P.tile([128, 4, 6], F32)
    mv = P.tile([128, 2], F32)
    rstd = P.tile([128, 1], F32)
    epsb = P.tile([128, 1], F32)
    P.memset(epsb.a, EPS)
    for t in range(NT):
        n = 1 if t == 0 else 0
        xt = xts[t % 2]; xn = xns[t % 2]; ht = hts[t % 2]
        P.dma(xt.a, x[t * 128:(t + 1) * 128, :])
        ln_tile_a(P, xt.a, xn.a, st, mv, rstd, epsb)
        for kc in range(16):
            pb = P.psum[1 + (kc // 4) % 4]
            P.transpose(pb[:, (kc % 4) * 128:(kc % 4 + 1) * 128], xn[:, kc * 128:(kc + 1) * 128], ident.a)
            if kc % 2 == 0:
                P.act(ht[:, kc, :], pb[:, (kc % 4) * 128:(kc % 4 + 1) * 128], AF.Identity,
                      bias=modT[:, kc, n:n + 1], scale=sc1p[:, kc, n:n + 1])
            else:
                P.ts(ht[:, kc, :], pb[:, (kc % 4) * 128:(kc % 4 + 1) * 128], sc1p[:, kc, n:n + 1], ALU.mult,
                     modT[:, kc, n:n + 1], ALU.add)
        P.dma(hT_o[:, :, t * 128:(t + 1) * 128].re("k p t -> p k t"), ht.a, eng="pool")
    return P.build()


class Rows:
    def __init__(self, tile, off):
        self.t = tile; self.off = off
    def __getitem__(self, key):
        return self.t[(key[0] + self.off,) + tuple(key[1:])]

def build_b():
    nc = bass.Bass("TRN2", target_bir_lowering=False)
    P = Prog(nc)
    I = "ExternalInput"
    hT_d = P.dram("hT", [16, 128, TOK], BF16, I)
    wf_d = P.dram("wf", [128, 16, 256], F32, I)
    cs_d = P.dram("cs", [128, 256], BF16, I)
    tab_d = P.dram("tab", [8, 32, 128, 2, 512], BF16, I)
    tabc_d = P.dram("tabc", [2, 128, 2, 256], BF16, I)
    hw = hgrn_decl(P)
    mw = mla_decl(P)
    mix_o = P.dram("mix", [8, 128, TOK], BF16, "ExternalOutput")
    fnet(P, hT_d, wf_d, cs_d, tab_d, tabc_d, Rows(mix_o, 0))
    hgrn(P, hT_d, *hw, Rows(mix_o, 2))
    mla(P, hT_d, *mw, Rows(mix_o, 4))
    return P.build()

def build_c():
    nc = bass.Bass("TRN2", target_bir_lowering=False)
    P = Prog(nc)
    ds = c_decl(P)
    phase_c(P, *ds)
    return P.build()

_CACHE = {}

def kernel(**inputs):
    d = {k: np.asarray(v) for k, v in inputs.items()}
    x_cur = d['x'].astype(np.float32).copy()
    ctx_cur = d['ctx'].astype(np.float32).copy()
    c = d['c']; c_ctx = d['c_ctx']
    if 'nc' not in _CACHE:
        _CACHE['nc'] = (build_a(), build_b(), build_c())
        _CACHE['fn'] = fnet_tables()
        _CACHE['hc'] = hgrn_consts()
        _CACHE['rt'] = rope_tables()
    ncA, ncB, ncC = _CACHE['nc']
    tab, tabc, cs = _CACHE['fn']
    identb, maskF, maskB, m01 = _CACHE['hc']
    COS2, SIN2 = _CACHE['rt']
    cores = list(range(8))
    identf = np.eye(128, dtype=np.float32)
    for l in range(4):
        xtoks = []
        in_maps = []
        bT = np.ascontiguousarray(d['b_ada'][l].reshape(96, 128).T)
        for core in cores:
            b, hf = core // 2, core % 2
            xt = np.ascontiguousarray(np.concatenate([ctx_cur[b, hf * 128:(hf + 1) * 128], x_cur[b, hf * 2048:(hf + 1) * 2048]], 0))
            xtoks.append(xt)
            cT = np.ascontiguousarray(np.stack([c[b].reshape(16, 128).T, c_ctx.reshape(16, 128).T], -1))
            in_maps.append({"x": xt, "cT": cT, "w_ada": d['w_ada'][l], "bT": bT, "ident": identf})
        resA = run_bass_kernel_spmd(ncA, in_maps, core_ids=cores).results
        in_maps = []
        wcache = {}
        for hf in range(2):
            w_in = d['w_in'][l]
            wf = np.ascontiguousarray(w_in[:, hf * 256:(hf + 1) * 256].reshape(16, 128, 256).transpose(1, 0, 2))
            m = {"wf": wf, "cs": cs, "tab": tab, "tabc": tabc, "identb": identb, "maskF": maskF, "maskB": maskB, "m01": m01,
                 "cos2": COS2, "sin2": SIN2}
            m.update(hgrn_weights(d, l, hf)); m.update(mla_weights(d, l, hf))
            wcache[hf] = m
        for core in cores:
            b, hf = core // 2, core % 2
            h0 = np.asarray(resA[2 * b]["hT"]); h1 = np.asarray(resA[2 * b + 1]["hT"])
            hfull = np.ascontiguousarray(np.concatenate([h0[:, :, :128], h1[:, :, :128], h0[:, :, 128:], h1[:, :, 128:]], -1))
            m = dict(wcache[hf]); m["hT"] = hfull
            in_maps.append(m)
        resB = run_bass_kernel_spmd(ncB, in_maps, core_ids=cores).results
        in_maps = []
        cw = c_weights(d, l)
        for core in cores:
            b, hf = core // 2, core % 2
            m0 = np.asarray(resB[2 * b]["mix"]); m1 = np.asarray(resB[2 * b + 1]["mix"])
            full = np.concatenate([m0[0:2], m1[0:2], m0[2:4], m1[2:4], m0[4:8], m1[4:8]], 0)
            mc = np.ascontiguousarray(np.concatenate([full[:, :, hf * 128:(hf + 1) * 128],
                                                      full[:, :, 256 + hf * 2048:256 + (hf + 1) * 2048]], -1))
            modT = np.asarray(resA[core]["modT"])
            bc1, bc2 = c_bcasts(d, l, modT)
            m = dict(cw); m.update({"x": xtoks[core], "mix": mc, "modT": modT, "bc1": bc1, "bc2": bc2})
            in_maps.append(m)
        resC = run_bass_kernel_spmd(ncC, in_maps, core_ids=cores).results
        for core in cores:
            b, hf = core // 2, core % 2
            o = np.asarray(resC[core]["out"])
            ctx_cur[b, hf * 128:(hf + 1) * 128] = o[:128]
            x_cur[b, hf * 2048:(hf + 1) * 2048] = o[128:]
    return x_cur.astype(np.float32)
```

```python
import time
import ml_dtypes

import numpy as np
from contextlib import ExitStack
import concourse.bass as bass
import concourse.mybir as mybir
from concourse.bass_utils import run_bass_kernel_spmd

F32 = mybir.dt.float32
BF16 = mybir.dt.bfloat16
ALU = mybir.AluOpType
AF = mybir.ActivationFunctionType
AX = mybir.AxisListType

SEM_CH = 4000
DMA_SLOTS = 8
DMA_USES = 240
SAME_ENG_SYNC = True
DT_SIZE = {F32: 4, BF16: 2}


class Buf:
    __slots__ = ("w", "r")

    def __init__(self):
        self.w = {}
        self.r = {}


class V:
    __slots__ = ("ap", "bufs")

    def __init__(self, ap, bufs):
        self.ap = ap
        self.bufs = bufs

    def __getitem__(self, key):
        return V(self.ap[key], self.bufs)

    def bc(self, shape):
        return V(self.ap.to_broadcast(shape), self.bufs)

    def re(self, s, **kw):
        return V(self.ap.rearrange(s, **kw), self.bufs)


class Tile:
    def __init__(self, ap, nsub=1):
        self.ap = ap
        self.nsub = nsub
        self.subs = [Buf() for _ in range(nsub)]
        self.shape = tuple(ap.shape)

    @property
    def a(self):
        return V(self.ap, self.subs)

    def __getitem__(self, key):
        return V(self.ap[key], self.subs)

    def q(self, i, w=128):
        return V(self.ap[:, i * w:(i + 1) * w], [self.subs[i]])

    def s(self, i, j=None):
        if self.nsub == 1:
            raise ValueError
        if j is None:
            return V(self.ap[:, i], [self.subs[i]])
        return V(self.ap[:, i:j], self.subs[i:j])


class Prog:
    ENGS = ("pe", "dve", "act", "pool", "sp")

    def __init__(self, nc):
        self.nc = nc
        self.es = ExitStack()
        self.ops = {e: [] for e in self.ENGS}
        self.cnt = {e: 0 for e in self.ENGS}
        self.dq = {e: 0 for e in self.ENGS}
        self.seen = {e: {} for e in self.ENGS}
        self.semkeys = set()
        self.last_dma = {}
        self.arena_bytes = 206 * 1024
        self.arena = self.es.enter_context(nc.sbuf_tensor("arena", [128, self.arena_bytes // 4], F32))
        self.top = 0
        self.ghosts = []
        self.live = []
        self.psum = []
        for i in range(8):
            t = self.es.enter_context(nc.psum_tensor(f"ps{i}", [128, 512], F32))
            self.psum.append(Tile(t[:], 4))
        self.ndram = 0

    def mark(self):
        return self.top

    def release(self, m):
        keep = []
        for (lo, hi, t) in self.live:
            if lo >= m:
                for b in t.subs:
                    self.ghosts.append((lo, hi, b))
            else:
                keep.append((lo, hi, t))
        self.live = keep
        self.top = m

    def tile(self, shape, dtype=F32, nsub=1, name=None):
        p = shape[0]
        free = int(np.prod(shape[1:]))
        nbytes = free * DT_SIZE[dtype]
        nbytes_al = (nbytes + 31) // 32 * 32
        lo = self.top
        hi = lo + nbytes_al
        if hi > self.arena_bytes:
            raise MemoryError(f"SBUF arena overflow: need {hi} for {name} {shape}")
        self.top = hi
        ap = self.arena[0:p, lo // 4:(lo + nbytes_al) // 4]
        if dtype != F32:
            ap = ap.bitcast(dtype)
        ap = ap[:, 0:free]
        if len(shape) > 2:
            names = " ".join(f"d{i}" for i in range(len(shape) - 1))
            kw = {f"d{i}": shape[i + 1] for i in range(len(shape) - 2)}
            ap = ap.rearrange(f"p ({names}) -> p {names}", **kw)
        t = Tile(ap, nsub)
        ng = []
        for (glo, ghi, b) in self.ghosts:
            if glo < hi and ghi > lo:
                for sb in t.subs:
                    for d in (b.w, b.r):
                        for k, v in d.items():
                            if sb.w.get(k, -1) < v:
                                sb.w[k] = v
                if glo < lo or ghi > hi:
                    ng.append((glo, ghi, b))
            else:
                ng.append((glo, ghi, b))
        self.ghosts = ng
        self.live.append((lo, hi, t))
        return t

    def dram(self, name, shape, dtype=F32, kind="Internal", nsub=1):
        h = self.nc.dram_tensor(name, list(shape), dtype, kind=kind)
        return Tile(h.ap(), nsub)

    def op(self, eng, fn, reads, writes, dma=False):
        toks = {}

        def add(d):
            for k, v in d.items():
                if toks.get(k, -1) < v:
                    toks[k] = v

        for v in reads:
            for b in v.bufs:
                add(b.w)
        for v in writes:
            for b in v.bufs:
                add(b.w)
                add(b.r)
        if dma:
            n = self.dq[eng]
            self.dq[eng] += 1
            j = n % DMA_SLOTS
            use = n // DMA_SLOTS
            if use > 0:
                pu = use - 1
                add({("q", f"q{eng}{j}_{pu // DMA_USES}"): 16 * (pu % DMA_USES + 1)})
            key = ("q", f"q{eng}{j}_{use // DMA_USES}")
            val = 16 * (use % DMA_USES + 1)
            self.semkeys.add(key[1])
            self.last_dma[key[1]] = val
            my = (key, val)
        else:
            i = self.cnt[eng]
            self.cnt[eng] += 1
            my = (("c", eng), i)
        deps = []
        for k, v in toks.items():
            if not dma and k == ("c", eng):
                if eng == "pe" or not SAME_ENG_SYNC:
                    continue
            deps.append((k, v))
        for v in reads:
            for b in v.bufs:
                if b.r.get(my[0], -1) < my[1]:
                    b.r[my[0]] = my[1]
        for v in writes:
            for b in v.bufs:
                b.w = {my[0]: my[1]}
                b.r = {}
        self.ops[eng].append((deps, fn, my, dma))

    def dma(self, out, in_, eng="sp"):
        self.op(eng, lambda e: e.dma_start(out=out.ap, in_=in_.ap), [in_], [out], dma=True)

    def mm(self, out, lhsT, rhs, start=True, stop=True):
        self.op("pe", lambda e: e.matmul(out.ap, lhsT.ap, rhs.ap, start=start, stop=stop),
                [lhsT, rhs], [out])

    def transpose(self, out, in_, ident):
        self.op("pe", lambda e: e.transpose(out.ap, in_.ap, ident.ap), [in_, ident], [out])

    def act(self, out, in_, func, bias=None, scale=None, accum_out=None, eng="act"):
        reads = [in_]
        kw = {}
        if bias is not None:
            if isinstance(bias, V):
                reads.append(bias)
                kw["bias"] = bias.ap
            else:
                kw["bias"] = float(bias)
        if scale is not None:
            if isinstance(scale, V):
                reads.append(scale)
                kw["scale"] = scale.ap
            else:
                kw["scale"] = float(scale)
        writes = [out]
        if accum_out is not None:
            writes.append(accum_out)
            kw["accum_out"] = accum_out.ap
        self.op("act", lambda e: e.activation(out.ap, in_.ap, func, **kw), reads, writes)

    def tt(self, out, in0, in1, op, eng="dve"):
        self.op(eng, lambda e: e.tensor_tensor(out.ap, in0.ap, in1.ap, op), [in0, in1], [out])

    def ts(self, out, in0, s1, op0, s2=None, op1=None, eng="dve"):
        reads = [in0]
        a1 = s1
        if isinstance(s1, V):
            reads.append(s1)
            a1 = s1.ap
        a2 = s2
        if isinstance(s2, V):
            reads.append(s2)
            a2 = s2.ap
        if op1 is None:
            self.op(eng, lambda e: e.tensor_scalar(out.ap, in0.ap, a1, None, op0), reads, [out])
        else:
            self.op(eng, lambda e: e.tensor_scalar(out.ap, in0.ap, a1, a2, op0, op1), reads, [out])

    def stt(self, out, in0, scalar, in1, op0, op1, eng="dve"):
        reads = [in0, in1]
        sc = scalar
        if isinstance(scalar, V):
            reads.append(scalar)
            sc = scalar.ap
        self.op(eng, lambda e: e.scalar_tensor_tensor(out.ap, in0.ap, sc, in1.ap, op0, op1), reads, [out])

    def copy(self, out, in_, eng="dve"):
        if eng == "act":
            self.op("act", lambda e: e.copy(out.ap, in_.ap), [in_], [out])
        else:
            self.op(eng, lambda e: e.tensor_copy(out.ap, in_.ap), [in_], [out])

    def memset(self, out, val, eng="dve"):
        self.op(eng, lambda e: e.memset(out.ap, val), [], [out])

    def generic(self, eng, fn, reads, writes):
        self.op(eng, fn, reads, writes)

    def build(self):
        nc = self.nc
        miles = {e: set() for e in self.ENGS}
        for e in self.ENGS:
            if e != "sp" and self.cnt[e] > 0:
                miles[e].add(self.cnt[e] - 1)
        for e in self.ENGS:
            for (deps, fn, my, dma) in self.ops[e]:
                for (k, v) in deps:
                    if k[0] == "c":
                        miles[k[1]].add(v)
        mnum = {}
        for e in self.ENGS:
            mnum[e] = {idx: n for n, idx in enumerate(sorted(miles[e]))}
            for n in range(len(miles[e])):
                self.semkeys.add(f"c{e}{n // SEM_CH}")

        def ctok(e, idx):
            n = mnum[e][idx]
            return (f"c{e}{n // SEM_CH}", n % SEM_CH + 1)

        fin = [(sk, val) for sk, val in self.last_dma.items()]
        for e in self.ENGS:
            if e != "sp" and self.cnt[e] > 0:
                fin.append(ctok(e, self.cnt[e] - 1))
        self.nwaits = 0
        with ExitStack() as es:
            sem = {k: es.enter_context(nc.semaphore(k)) for k in sorted(self.semkeys)}
            block = es.enter_context(nc.Block())

            def mk(name):
                def body(e):
                    seen = {}
                    for (deps, fn, my, dma) in self.ops[name]:
                        for (k, v) in deps:
                            if seen.get(k, -1) >= v:
                                continue
                            seen[k] = v
                            if k[0] == "c":
                                sk, sv = ctok(k[1], v)
                            else:
                                sk, sv = k[1], v
                            e.wait_ge(sem[sk], sv)
                            self.nwaits += 1
                        ins = fn(e)
                        if dma:
                            ins.then_inc(sem[my[0][1]], 16)
                        elif my[1] in mnum[name]:
                            sk, sv = ctok(name, my[1])
                            ins.then_inc(sem[sk], 1)
                    if name == "sp":
                        for (wk, wv) in fin:
                            e.wait_ge(sem[wk], wv)
                return body

            block.tensor(mk("pe"))
            block.vector(mk("dve"))
            block.scalar(mk("act"))
            block.gpsimd(mk("pool"))
            block.sync(mk("sp"))
        self.es.close()
        return nc

import ml_dtypes
BF = ml_dtypes.bfloat16
TOK = 4352
EPS = 1e-6

def seg_range(s):
    return (0, 256) if s == 0 else (256 + (s - 1) * 512, 512)

def load_cast(P, dst, src, stg, shape1):
    A, B = shape1
    step = max(1, stg[0].shape[1] // B)
    i = 0
    k = getattr(P, "_lc", 0)
    while i < A:
        n = min(step, A - i)
        st = stg[k % len(stg)]
        sv = st[:, 0:n * B].re("p (a b) -> p a b", b=B)
        P.dma(sv, src[:, i:i + n, :])
        P.copy(dst[:, i:i + n, :], sv, eng=("dve" if k % 2 == 0 else "pool"))
        i += n
        k += 1
    P._lc = k

def load_hT(P, hT_d, tok0, n, buf):
    P.dma(buf[:, :, 0:n], hT_d[:, :, tok0:tok0 + n].re("k p t -> p k t"))

def proj(P, out, W, c0, M, hbuf, n):
    for kc in range(16):
        P.mm(out, W[:, kc, c0:c0 + M], hbuf[:, kc, 0:n], start=(kc == 0), stop=(kc == 15))


def fnet(P, hT_d, wf_d, cs_d, tab_d, tabc_d, mix_o):
    m0 = P.mark()
    Wf = P.tile([128, 16, 256], BF16)
    cs = P.tile([128, 256], BF16)
    P.dma(cs.a, cs_d.a)
    AB = [P.tile([128, 34, 256], BF16, nsub=34) for _ in range(2)]
    hb = [P.tile([128, 16, 512], BF16) for _ in range(2)]
    m1 = P.mark()
    stg = [P.tile([128, 4096], F32) for _ in range(2)]
    load_cast(P, Wf.a, wf_d.a, stg, (16, 256))
    P.release(m1)
    UT = [P.tile([128, 512], BF16) for _ in range(2)]
    for s in range(9):
        t0, n = seg_range(s)
        h = hb[s % 2]
        load_hT(P, hT_d, t0, n, h)
        for g in range(2):
            ps = P.psum[g]
            proj(P, ps[:, 0:n], Wf, g * 128, 128, h, n)
            P.copy(UT[g][:, 0:n], ps[:, 0:n], eng="act")
            for tt in range(n // 128):
                pa = P.psum[2 + (tt % 2)]
                P.mm(pa[:, 0:256], UT[g][:, tt * 128:(tt + 1) * 128], cs.a)
                P.copy(AB[g].s(t0 // 128 + tt), pa[:, 0:256], eng=("dve" if tt % 2 == 0 else "act"))
    tb = [P.tile([128, 2, 512], BF16) for _ in range(3)]
    ob = [P.tile([128, 512], BF16) for _ in range(2)]
    k = 0
    for tt in range(2):
        t = tb[k % 3]; k += 1
        P.dma(t[:, :, 0:256], tabc_d[tt])
        for g in range(2):
            P.mm(P.psum[4 + g][:, 0:256], AB[g].s(tt)[:, 0:128], t[:, 0, 0:256], start=(tt == 0), stop=False)
            P.mm(P.psum[4 + g][:, 0:256], AB[g].s(tt)[:, 128:256], t[:, 1, 0:256], start=False, stop=(tt == 1))
    for g in range(2):
        P.copy(ob[g][:, 0:256], P.psum[4 + g][:, 0:256], eng="act")
        P.dma(mix_o[g, :, 0:256], ob[g][:, 0:256], eng="pool")
    for blk in range(8):
        for tt in range(32):
            t = tb[k % 3]; k += 1
            P.dma(t.a, tab_d[blk, tt])
            for g in range(2):
                pz = P.psum[4 + 2 * (blk % 2) + g]
                P.mm(pz.a, AB[g].s(2 + tt)[:, 0:128], t[:, 0, :], start=(tt == 0), stop=False)
                P.mm(pz.a, AB[g].s(2 + tt)[:, 128:256], t[:, 1, :], start=False, stop=(tt == 31))
        for g in range(2):
            pz = P.psum[4 + 2 * (blk % 2) + g]
            P.copy(ob[g].a, pz.a, eng=("act" if g == 0 else "dve"))
            P.dma(mix_o[g, :, 256 + blk * 512:256 + (blk + 1) * 512], ob[g].a, eng="pool")
    P.release(m0)

def fnet_tables():
    T = 4096
    t = np.arange(T, dtype=np.int64)
    m = (t[:, None] * t[None, :]) % T
    ang = 2 * np.pi * m / T
    sc = 1.0 / np.sqrt(T * 128)
    C = (np.cos(ang) * sc).astype(np.float32); S = (-np.sin(ang) * sc).astype(np.float32)
    tab = np.stack([C, S], 0).reshape(2, 32, 128, 8, 512).transpose(3, 1, 2, 0, 4)
    tab = np.ascontiguousarray(tab).astype(BF)
    T2 = 256
    t = np.arange(T2)
    ang = 2 * np.pi * ((t[:, None] * t[None, :]) % T2) / T2
    sc = 1.0 / np.sqrt(T2 * 128)
    Cc = np.cos(ang) * sc; Sc = -np.sin(ang) * sc
    tabc = np.ascontiguousarray(np.stack([Cc, Sc], 0).reshape(2, 2, 128, 256).transpose(1, 2, 0, 3)).astype(BF)
    c = np.arange(128)
    ang = 2 * np.pi * ((c[:, None] * c[None, :]) % 128) / 128
    cs = np.concatenate([np.cos(ang), np.sin(ang)], 1).astype(BF)
    return tab, tabc, cs


CH = 16
import os
STOP = int(os.environ.get('STOP', '99'))

def hgrn_consts():
    identb = np.eye(128, dtype=np.float32).astype(BF)
    s = np.arange(CH)[:, None]; t = np.arange(512)[None, :] % CH
    maskF = (s <= t).astype(np.float32); maskB = (s >= t).astype(np.float32)
    m01 = np.ones((128, 512), np.float32); m01[:, 0::CH] = 0.0
    return identb, np.ascontiguousarray(maskF), np.ascontiguousarray(maskB), m01

def hgrn(P, hT_d, wh_d, lbl_d, msk_d, gn_d, identb_d, maskF_d, maskB_d, m01_d, mix_o):
    m0 = P.mark()
    ident = P.tile([128, 128], BF16); P.dma(ident.a, identb_d.a)
    ones = P.tile([128, 128], BF16); P.memset(ones.a, 1.0)
    epsb = P.tile([128, 1], F32); P.memset(epsb.a, EPS)
    masks = [P.tile([CH, 512], F32), P.tile([CH, 512], F32)]
    P.dma(masks[0].a, maskF_d.a); P.dma(masks[1].a, maskB_d.a)
    m01 = P.tile([128, 512], F32); P.dma(m01.a, m01_d.a)
    gn = P.tile([128, 2], F32); P.dma(gn.a, gn_d.a)
    lbl = P.tile([128, 4, 4], F32); P.dma(lbl.a, lbl_d.a)
    msk = P.tile([128, 4], F32); P.dma(msk.a, msk_d.a)
    el = P.tile([128, 4, 4], F32); P.act(el.a, lbl.a, AF.Exp)
    se = P.tile([128, 4], F32)
    P.op("dve", lambda e: e.reduce_sum(se.ap, el.ap, AX.X), [el.a], [se.a])
    em = P.tile([128, 4, 4], F32)
    P.tt(em.a, el.a, msk.a.re("p (o j) -> p o j", o=1).bc([128, 4, 4]), ALU.mult)
    num = P.tile([128, 4], F32)
    P.op("dve", lambda e: e.reduce_sum(num.ap, em.ap, AX.X), [em.a], [num.a])
    P.op("dve", lambda e: e.reciprocal(se.ap, se.ap), [se.a], [se.a])
    lb = P.tile([128, 4], F32); P.tt(lb.a, num.a, se.a, ALU.mult)
    oml = P.tile([128, 4], F32); P.ts(oml.a, lb.a, -1.0, ALU.mult, 1.0, ALU.add)

    if STOP == 1:
        return
    W = P.tile([128, 16, 640], BF16)
    hb = [P.tile([128, 16, 512], BF16) for _ in range(2)]
    oacc = P.tile([128, TOK], F32)
    sg = P.tile([128, TOK], BF16)
    S = P.tile([128, 128], F32); Sb = P.tile([128, 128], BF16)
    def tset():
        return dict(f=P.tile([128, 512], F32), logf=P.tile([128, 512], F32), k=P.tile([128, 512], F32),
                    a=P.tile([128, 512], F32), a2=P.tile([128, 512], F32), ea=P.tile([128, 512], F32), ena=P.tile([128, 512], F32),
                    ke=P.tile([128, 512], F32), qe=P.tile([128, 512], BF16), keb=P.tile([128, 512], BF16),
                    kdb=P.tile([128, 512], BF16), ib=P.tile([128, 512], BF16),
                    vtok=P.tile([CH, 512 // CH, 128], BF16, nsub=512 // CH), kdtok=P.tile([CH, 512 // CH, 128], BF16, nsub=512 // CH), scm=P.tile([CH, 512], BF16))
    sets = [tset(), tset()]
    m1 = P.mark()
    stg = [P.tile([128, 4096], F32) for _ in range(2)]
    tmp = [P.tile([128, 512], F32) for _ in range(2)]
    ob = [P.tile([128, 512], BF16) for _ in range(2)]

    def prep(T, s, d, hh, h):
        t0, n = seg_range(s)
        ncx = n // CH
        load_hT(P, hT_d, t0, n, h)
        pq, pi, pz, pg = P.psum[0], P.psum[1], P.psum[2], P.psum[3]
        proj(P, pq[:, 0:n], W, 0, 128, h, n)
        proj(P, pi[:, 0:n], W, 128, 128, h, n)
        proj(P, pz[:, 0:n], W, (3 + d) * 128, 128, h, n)
        if d == 1:
            proj(P, pg[:, 0:n], W, 256, 128, h, n)
            P.act(sg[:, t0:t0 + n], pg[:, 0:n], AF.Silu)
        ci = d * 2 + hh
        P.act(T["f"][:, 0:n], pz[:, 0:n], AF.Sigmoid)
        P.ts(T["f"][:, 0:n], T["f"][:, 0:n], oml[:, ci:ci + 1], ALU.mult, lb[:, ci:ci + 1], ALU.add)
        P.act(T["logf"][:, 0:n], T["f"][:, 0:n], AF.Ln)
        P.ts(T["k"][:, 0:n], T["f"][:, 0:n], -1.0, ALU.mult, 1.0, ALU.add, eng="pool")
        P.op("dve", lambda e: e.tensor_tensor_scan(T["a"].ap[:, 0:n], m01.ap[:, 0:n], T["logf"].ap[:, 0:n], 0.0, ALU.mult, ALU.add),
             [m01.a, T["logf"].a], [T["a"].a])
        a = T["a"]
        if d == 1:
            a3 = T["a"][:, 0:n].re("p (c t) -> p c t", t=CH)
            P.tt(T["a2"][:, 0:n], T["logf"][:, 0:n], T["a"][:, 0:n], ALU.subtract)
            P.tt(T["a2"][:, 0:n].re("p (c t) -> p c t", t=CH), T["a2"][:, 0:n].re("p (c t) -> p c t", t=CH),
                 a3[:, :, CH - 1:CH].bc([128, ncx, CH]), ALU.add)
            a = T["a2"]
        P.ts(a[:, 0:n], a[:, 0:n], -80.0, ALU.max)
        P.act(T["ea"][:, 0:n], a[:, 0:n], AF.Exp)
        P.act(T["ena"][:, 0:n], a[:, 0:n], AF.Exp, scale=-1.0)
        P.tt(T["qe"][:, 0:n], pq[:, 0:n], T["ea"][:, 0:n], ALU.mult)
        P.tt(T["ke"][:, 0:n], T["k"][:, 0:n], T["ena"][:, 0:n], ALU.mult)
        P.copy(T["keb"][:, 0:n], T["ke"][:, 0:n], eng="pool")
        ea3 = T["ea"][:, 0:n].re("p (c t) -> p c t", t=CH)
        eend = ea3[:, :, CH - 1:CH] if d == 0 else ea3[:, :, 0:1]
        P.tt(T["kdb"][:, 0:n].re("p (c t) -> p c t", t=CH), T["ke"][:, 0:n].re("p (c t) -> p c t", t=CH),
             eend.bc([128, ncx, CH]), ALU.mult)
        P.copy(T["ib"][:, 0:n], pi[:, 0:n], eng="act")
        pt = P.psum[4]
        for c0 in range(0, ncx, 4):
            for c in range(c0, c0 + 4):
                P.mm(pt[0:CH, (c - c0) * 128:(c - c0 + 1) * 128], T["ib"][:, c * CH:(c + 1) * CH], ident.a)
            P.copy(T["vtok"].s(c0, c0 + 4), pt[0:CH, :].re("p (a b) -> p a b", b=128), eng="act")
            for c in range(c0, c0 + 4):
                P.mm(pt[0:CH, (c - c0) * 128:(c - c0 + 1) * 128], T["kdb"][:, c * CH:(c + 1) * CH], ident.a)
            P.copy(T["kdtok"].s(c0, c0 + 4), pt[0:CH, :].re("p (a b) -> p a b", b=128), eng="dve")
        psc = P.psum[5]
        for c in range(ncx):
            P.mm(psc[0:CH, c * CH:(c + 1) * CH], T["keb"][:, c * CH:(c + 1) * CH], T["qe"][:, c * CH:(c + 1) * CH])
        P.tt(T["scm"][:, 0:n], psc[0:CH, 0:n], masks[d][:, 0:n], ALU.mult)

    def loop(T, s, d):
        t0, n = seg_range(s)
        ncx = n // CH
        pO = P.psum[6]; pSs = [P.psum[7], P.psum[3]]
        order = range(ncx) if d == 0 else range(ncx - 1, -1, -1)
        for ii, c in enumerate(order):
            cs = slice(c * CH, (c + 1) * CH)
            P.mm(pO[:, cs], T["vtok"].s(c), T["scm"][:, cs], start=True, stop=False)
            pS = pSs[ii % 2]
            P.mm(pS[:, 0:128], T["kdtok"].s(c), T["vtok"].s(c))
            P.mm(pO[:, cs], Sb.a, T["qe"][:, cs], start=False, stop=True)
            col = c * CH + CH - 1 if d == 0 else c * CH
            P.stt(S.a, S.a, T["ea"][:, col:col + 1], pS[:, 0:128], ALU.mult, ALU.add)
            P.copy(Sb.a, S.a, eng="act")
        if d == 0:
            P.copy(oacc[:, t0:t0 + n], pO[:, 0:n], eng="act")
        else:
            P.tt(oacc[:, t0:t0 + n], oacc[:, t0:t0 + n], pO[:, 0:n], ALU.add)

    for hh in range(2):
        load_cast(P, W.a.re("p k (j c) -> p k j c", j=5), wh_d[:, :, :, hh * 128:(hh + 1) * 128], stg, (16, 640)) if False else None
        for j in range(5):
            for half in range(2):
                st = stg[(j * 2 + half) % 2]
                sv = st[:, 0:1024].re("p (a b) -> p a b", b=128)
                P.dma(sv, wh_d[:, half * 8:(half + 1) * 8, j, hh * 128:(hh + 1) * 128])
                P.copy(W[:, half * 8:(half + 1) * 8, j * 128:(j + 1) * 128], sv, eng=("dve" if half == 0 else "pool"))
        for d in range(2):
            P.memset(S.a, 0.0); P.memset(Sb.a, 0.0)
            segs = list(range(9)) if d == 0 else [0] + list(range(8, 0, -1))
            prep(sets[0], segs[0], d, hh, hb[0])
            if STOP == 2:
                return
            for i, s in enumerate(segs):
                if i + 1 < len(segs):
                    prep(sets[(i + 1) % 2], segs[i + 1], d, hh, hb[(i + 1) % 2])
                loop(sets[i % 2], s, d)
                if STOP == 3:
                    return
        for s in range(9):
            t0, n = seg_range(s)
            sq = sets[0]["qe"]
            P.act(sq[:, 0:n], oacc[:, t0:t0 + n], AF.Square)
            pss = P.psum[s % 2]
            P.mm(pss[:, 0:n], ones.a, sq[:, 0:n])
            r = tmp[s % 2]
            P.act(r[:, 0:n], pss[:, 0:n], AF.Sqrt, bias=epsb.a, scale=1.0 / 128)
            P.op("dve", lambda e, r=r, n=n: e.reciprocal(r.ap[:, 0:n], r.ap[:, 0:n]), [r.a], [r.a])
            P.tt(r[:, 0:n], r[:, 0:n], oacc[:, t0:t0 + n], ALU.mult)
            P.tt(r[:, 0:n], r[:, 0:n], sg[:, t0:t0 + n], ALU.mult, eng="pool")
            o = ob[s % 2]
            P.ts(o[:, 0:n], r[:, 0:n], gn[:, hh:hh + 1], ALU.mult)
            P.dma(mix_o[hh, :, t0:t0 + n], o[:, 0:n], eng="pool")
    P.release(m0)

def hgrn_weights(d, l, hf):
    w_in = d['w_in'][l]
    wh = w_in[:, 512:512 + 5 * 512].reshape(2048, 5, 4, 128)[:, :, hf * 2:(hf + 1) * 2].reshape(2048, 5, 256)
    wh = np.ascontiguousarray(wh.reshape(16, 128, 5, 256).transpose(1, 0, 2, 3))
    lg = d['hgrn_lb_logits']
    lbl = lg.reshape(4, 2, 4, 128)[:, :, hf * 2:(hf + 1) * 2]
    lbl = np.ascontiguousarray(lbl.transpose(3, 1, 2, 0).reshape(128, 4, 4))
    msk = np.zeros((128, 4), np.float32); msk[:, 1:l + 1] = 1.0
    gn = np.ascontiguousarray(d['hgrn_norm'][l].reshape(4, 128)[hf * 2:(hf + 1) * 2].T)
    return dict(wh=wh, lbl=lbl, msk=msk, gn=gn)

def hgrn_decl(P):
    return [P.dram("wh", [128, 16, 5, 256], F32, "ExternalInput"), P.dram("lbl", [128, 4, 4], F32, "ExternalInput"),
            P.dram("msk", [128, 4], F32, "ExternalInput"), P.dram("gn", [128, 2], F32, "ExternalInput"),
            P.dram("identb", [128, 128], BF16, "ExternalInput"), P.dram("maskF", [CH, 512], F32, "ExternalInput"),
            P.dram("maskB", [CH, 512], F32, "ExternalInput"), P.dram("m01", [128, 512], F32, "ExternalInput")]


import os
STOP = int(os.environ.get('STOP', '99'))
MLA_SCALE = 192 ** -0.5

def rmsnorm_fm(P, ps_list, n, nch, dim, g, out, seg0, sq, ones, pss, rstd, epsb):
    for j in range(nch):
        P.act(sq[:, j, 0:n], ps_list[j], AF.Square)
    for j in range(nch):
        P.mm(pss[:, 0:n], ones.a, sq[:, j, 0:n], start=(j == 0), stop=(j == nch - 1))
    P.act(rstd[:, 0:n], pss[:, 0:n], AF.Sqrt, bias=epsb.a, scale=1.0 / dim)
    P.op("dve", lambda e: e.reciprocal(rstd.ap[:, 0:n], rstd.ap[:, 0:n]), [rstd.a], [rstd.a])
    for j in range(nch):
        P.stt(out[:, j, seg0:seg0 + n], ps_list[j], g[:, j:j + 1], rstd[:, 0:n], ALU.mult, ALU.mult)

def rope_fm(P, out, a_ps, b_ps, cos, sin, n, t1, t2):
    P.tt(t1[:, 0:n], a_ps, cos[:, 0:n], ALU.mult)
    P.tt(t2[:, 0:n], b_ps, sin[:, 0:n], ALU.mult)
    P.tt(out, t1[:, 0:n], t2[:, 0:n], ALU.add, eng="pool")

def mla(P, hT_d, wq_d, wkv_d, wkr_d, wuq_d, wuk_d, wuv_d, gq_d, gkv_d, cos_d, sin_d, mix_o):
    m0 = P.mark()
    ones = P.tile([128, 128], BF16); P.memset(ones.a, 1.0)
    epsb = P.tile([128, 1], F32); P.memset(epsb.a, EPS)
    gq = P.tile([128, 4], F32); P.dma(gq.a, gq_d.a)
    gkv = P.tile([128, 2], F32); P.dma(gkv.a, gkv_d.a)
    cqn = P.tile([128, 4, TOK], BF16)
    ckvn = P.tile([128, 2, TOK], BF16)
    KR = P.tile([64, TOK], BF16)
    Wuq = P.tile([128, 4, 1024], BF16)
    Wuk = P.tile([128, 2, 512], BF16)
    Wuv = P.tile([128, 2, 512], BF16)
    cosb = [P.tile([64, 512], F32) for _ in range(2)]
    sinb = [P.tile([64, 512], F32) for _ in range(2)]
    t1 = P.tile([64, 512], F32); t2 = P.tile([64, 512], F32)
    rstd = P.tile([128, 512], F32)
    m1 = P.mark()
    Wq = P.tile([128, 16, 512], BF16)
    Wkv = P.tile([128, 16, 256], BF16)
    Wkr = P.tile([128, 16, 128], BF16)
    hb = [P.tile([128, 16, 512], BF16) for _ in range(2)]
    sq = P.tile([128, 4, 512], BF16)
    stg = [P.tile([128, 4096], F32) for _ in range(2)]
    load_cast(P, Wq.a, wq_d.a, stg, (16, 512))
    load_cast(P, Wkv.a, wkv_d.a, stg, (16, 256))
    load_cast(P, Wkr.a, wkr_d.a, stg, (16, 128))
    load_cast(P, Wuq.a, wuq_d.a, stg, (4, 1024))
    load_cast(P, Wuk.a, wuk_d.a, stg, (2, 512))
    load_cast(P, Wuv.a, wuv_d.a, stg, (2, 512))
    for s in range(9):
        t0, n = seg_range(s)
        h = hb[s % 2]
        load_hT(P, hT_d, t0, n, h)
        if s > 0:
            P.dma(cosb[s % 2].a, cos_d[:, (s - 1) * 512:s * 512])
            P.dma(sinb[s % 2].a, sin_d[:, (s - 1) * 512:s * 512])
        for j in range(4):
            proj(P, P.psum[j][:, 0:n], Wq, j * 128, 128, h, n)
        rmsnorm_fm(P, [P.psum[j][:, 0:n] for j in range(4)], n, 4, 512, gq, cqn, t0, sq, ones, P.psum[4], rstd, epsb)
        for j in range(2):
            proj(P, P.psum[5 + j][:, 0:n], Wkv, j * 128, 128, h, n)
        rmsnorm_fm(P, [P.psum[5 + j][:, 0:n] for j in range(2)], n, 2, 256, gkv, ckvn, t0, sq, ones, P.psum[7], rstd, epsb)
        proj(P, P.psum[0][0:64, 0:n], Wkr, 0, 64, h, n)
        if s == 0:
            P.copy(KR[:, t0:t0 + n], P.psum[0][0:64, 0:n], eng="act")
        else:
            proj(P, P.psum[1][0:64, 0:n], Wkr, 64, 64, h, n)
            rope_fm(P, KR[:, t0:t0 + n], P.psum[0][0:64, 0:n], P.psum[1][0:64, 0:n], cosb[s % 2], sinb[s % 2], n, t1, t2)
    P.release(m1)
    if STOP == 1:
        return
    QN = P.tile([128, TOK], BF16); QR = P.tile([64, TOK], BF16); KN = P.tile([128, TOK], BF16)
    Vh = P.tile([128, 34, 128], BF16)
    PT = [P.tile([128, 512], BF16) for _ in range(3)]
    rden = P.tile([128, 512], F32)
    ob = [P.tile([128, 512], BF16) for _ in range(2)]
    for hd in range(4):
        for s in range(9):
            t0, n = seg_range(s)
            if s > 0:
                P.dma(cosb[s % 2].a, cos_d[:, (s - 1) * 512:s * 512])
                P.dma(sinb[s % 2].a, sin_d[:, (s - 1) * 512:s * 512])
            ps = P.psum[0]
            for cc in range(4):
                P.mm(ps[:, 0:n], Wuq[:, cc, hd * 256:hd * 256 + 128], cqn[:, cc, t0:t0 + n], start=(cc == 0), stop=(cc == 3))
            P.copy(QN[:, t0:t0 + n], ps[:, 0:n], eng="act")
            ps1 = P.psum[1]; ps2 = P.psum[2]
            for cc in range(4):
                P.mm(ps1[0:64, 0:n], Wuq[:, cc, hd * 256 + 128:hd * 256 + 192], cqn[:, cc, t0:t0 + n], start=(cc == 0), stop=(cc == 3))
            if s == 0:
                P.copy(QR[:, t0:t0 + n], ps1[0:64, 0:n], eng="act")
            else:
                for cc in range(4):
                    P.mm(ps2[0:64, 0:n], Wuq[:, cc, hd * 256 + 192:hd * 256 + 256], cqn[:, cc, t0:t0 + n], start=(cc == 0), stop=(cc == 3))
                rope_fm(P, QR[:, t0:t0 + n], ps1[0:64, 0:n], ps2[0:64, 0:n], cosb[s % 2], sinb[s % 2], n, t1, t2)
            ps3 = P.psum[3]
            for cc in range(2):
                P.mm(ps3[:, 0:n], Wuk[:, cc, hd * 128:(hd + 1) * 128], ckvn[:, cc, t0:t0 + n], start=(cc == 0), stop=(cc == 1))
            P.copy(KN[:, t0:t0 + n], ps3[:, 0:n], eng="dve")
            ps4 = P.psum[4]
            for tt in range(n // 128):
                for cc in range(2):
                    P.mm(ps4[:, tt * 128:(tt + 1) * 128], ckvn[:, cc, t0 + tt * 128:t0 + (tt + 1) * 128],
                         Wuv[:, cc, hd * 128:(hd + 1) * 128], start=(cc == 0), stop=(cc == 1))
            P.copy(Vh[:, t0 // 128:t0 // 128 + n // 128, :], ps4[:, 0:n].re("p (a b) -> p a b", b=128), eng="act")
        if STOP == 2:
            return
        k = 0
        for qb in range(9):
            q0, nq = seg_range(qb)
            nkt = 2 if qb == 0 else 34
            pO = P.psum[5]; pD = P.psum[6]
            for kt in range(nkt):
                pS = P.psum[kt % 2 + (0 if qb % 2 == 0 else 2)]
                P.mm(pS[:, 0:nq], KN[:, kt * 128:(kt + 1) * 128], QN[:, q0:q0 + nq], start=True, stop=False)
                P.mm(pS[:, 0:nq], KR[:, kt * 128:(kt + 1) * 128], QR[:, q0:q0 + nq], start=False, stop=True)
                pt = PT[k % 3]; k += 1
                P.act(pt[:, 0:nq], pS[:, 0:nq], AF.Exp, scale=MLA_SCALE)
                P.mm(pO[:, 0:nq], Vh[:, kt, :], pt[:, 0:nq], start=(kt == 0), stop=(kt == nkt - 1))
                P.mm(pD[:, 0:nq], ones.a, pt[:, 0:nq], start=(kt == 0), stop=(kt == nkt - 1))
            P.op("dve", lambda e, nq=nq: e.reciprocal(rden.ap[:, 0:nq], pD.ap[:, 0:nq]), [pD.a], [rden.a])
            o = ob[qb % 2]
            P.tt(o[:, 0:nq], pO[:, 0:nq], rden[:, 0:nq], ALU.mult)
            P.dma(mix_o[hd, :, q0:q0 + nq], o[:, 0:nq], eng="pool")
            if STOP == 3:
                return
    P.release(m0)

def rope_tables():
    t = np.arange(4096)
    row, col = t // 64, t % 64
    inv = 10000.0 ** (-np.arange(16, dtype=np.float32) / 16)
    ang = np.concatenate([row[:, None] * inv, col[:, None] * inv], -1)
    cos, sin = np.cos(ang).T, np.sin(ang).T
    COS2 = np.concatenate([cos, cos], 0).astype(np.float32)
    SIN2 = np.concatenate([-sin, sin], 0).astype(np.float32)
    return np.ascontiguousarray(COS2), np.ascontiguousarray(SIN2)

PERM = np.concatenate([np.arange(0, 64, 2), np.arange(1, 64, 2)])
PERM_SW = np.concatenate([np.arange(1, 64, 2), np.arange(0, 64, 2)])

def mla_weights(d, l, hf):
    w_in = d['w_in'][l]
    o = 512 + 5 * 512
    wq = w_in[:, o:o + 512]; wkv = w_in[:, o + 512:o + 768]; wkr = w_in[:, o + 768:o + 832]
    wkr2 = np.concatenate([wkr[:, PERM], wkr[:, PERM_SW]], 1)
    fm = lambda w: np.ascontiguousarray(w.reshape(w.shape[0] // 128, 128, -1).transpose(1, 0, 2))
    wuq = d['w_uq'][l].reshape(512, 8, 192)[:, hf * 4:(hf + 1) * 4]
    wuq2 = np.concatenate([wuq[:, :, :128], wuq[:, :, 128:][:, :, PERM], wuq[:, :, 128:][:, :, PERM_SW]], -1).reshape(512, 1024)
    wukv = d['w_ukv'][l].reshape(256, 8, 256)[:, hf * 4:(hf + 1) * 4]
    wuk = wukv[:, :, :128].reshape(256, 512); wuv = wukv[:, :, 128:].reshape(256, 512)
    return dict(wq=fm(wq), wkv=fm(wkv), wkr=fm(wkr2), wuq=fm(wuq2), wuk=fm(wuk), wuv=fm(wuv),
                gq=np.ascontiguousarray(d['mla_q_norm'][l].reshape(4, 128).T), gkv=np.ascontiguousarray(d['mla_kv_norm'][l].reshape(2, 128).T))

def mla_decl(P):
    return [P.dram("wq", [128, 16, 512], F32, "ExternalInput"), P.dram("wkv", [128, 16, 256], F32, "ExternalInput"),
            P.dram("wkr", [128, 16, 128], F32, "ExternalInput"), P.dram("wuq", [128, 4, 1024], F32, "ExternalInput"),
            P.dram("wuk", [128, 2, 512], F32, "ExternalInput"), P.dram("wuv", [128, 2, 512], F32, "ExternalInput"),
            P.dram("gq", [128, 4], F32, "ExternalInput"), P.dram("gkv", [128, 2], F32, "ExternalInput"),
            P.dram("cos2", [64, 4096], F32, "ExternalInput"), P.dram("sin2", [64, 4096], F32, "ExternalInput")]


NT = 17
ALPHA = 8 ** 0.25
GROUPS = [(0, 6), (6, 12), (12, 17)]

def ln_tile(P, xt, xn, st, mv, rstd, epsb):
    for c in range(4):
        P.op("dve", lambda e, c=c: e.bn_stats(st.ap[:, c, :], xt.ap[:, c * 512:(c + 1) * 512]), [xt], [st.a])
    P.op("dve", lambda e: e.bn_aggr(mv.ap, st.ap.rearrange("p a b -> p (a b)")), [st.a], [mv.a])
    P.act(rstd.a, mv[:, 1:2], AF.Sqrt, bias=epsb.a)
    P.op("dve", lambda e: e.reciprocal(rstd.ap, rstd.ap), [rstd.a], [rstd.a])
    P.ts(xn, xt, mv[:, 0:1], ALU.subtract, rstd.a, ALU.mult)

def phase_c(P, x_d, mix_d, wout_d, modT_d, bc1_d, bc2_d, wr_d, rbb_d, w1_d, b1g_d, b1l_d, w2_d, b2_d, identf_d,
            x1_d, h2_d, out_d):
    epsb = P.tile([128, 1], F32); P.memset(epsb.a, EPS)
    identf = P.tile([128, 128], F32); P.dma(identf.a, identf_d.a)
    modT = P.tile([128, 96, 2], F32); P.dma(modT.a, modT_d.a)
    sc2p = P.tile([128, 16, 2], F32); P.ts(sc2p.a, modT[:, 64:80, :], 1.0, ALU.add)
    G = P.tile([128, NT, 32], F32)
    GT = P.tile([32, NT * 128], F32)
    st = P.tile([128, 4, 6], F32); mv = P.tile([128, 2], F32); rstd = P.tile([128, 1], F32)
    b1g = P.tile([128, 32, 6], F32); P.dma(b1g.a, b1g_d.a)
    b1l = P.tile([128, 32, 6], F32); P.dma(b1l.a, b1l_d.a)
    m0 = P.mark()
    Wout = P.tile([128, 16, 2048], BF16)
    bc = [P.tile([128, 2048], F32) for _ in range(4)]
    for i in range(4):
        P.dma(bc[i].a, V(bc1_d.ap[i].partition_broadcast(128), bc1_d.subs))
    Wr = P.tile([128, 16, 32], F32); P.dma(Wr.a, wr_d.a)
    rbb = P.tile([128, 32], F32); P.dma(rbb.a, rbb_d.a)
    mx = [P.tile([128, 16, 128], BF16) for _ in range(2)]
    xt = [P.tile([128, 2048], F32) for _ in range(2)]
    yt = P.tile([128, 2048], F32)
    x1 = [P.tile([128, 2048], F32) for _ in range(2)]
    h2b = [P.tile([128, 16, 128], BF16) for _ in range(2)]
    h2f = P.tile([128, 16, 128], F32)
    lg = P.tile([128, 32], F32); m8 = P.tile([128, 8], F32); negm = P.tile([128, 1], F32)
    mk = P.tile([128, 32], F32); ex = P.tile([128, 32], F32); ssum = P.tile([128, 1], F32)
    m1 = P.mark()
    stg = [P.tile([128, 2048], F32) for _ in range(2)]
    load_cast(P, Wout.a, wout_d.a, stg, (16, 2048))
    P.release(m1)
    for t in range(NT):
        n = 1 if t == 0 else 0
        ts_ = slice(t * 128, (t + 1) * 128)
        P.dma(mx[t % 2].a, mix_d[:, :, ts_].re("k p t -> p k t"))
        P.dma(xt[t % 2].a, x_d[ts_, :])
        for db in range(4):
            for fc in range(16):
                P.mm(P.psum[db].a, mx[t % 2][:, fc, :], Wout[:, fc, db * 512:(db + 1) * 512], start=(fc == 0), stop=(fc == 15))
            P.tt(yt[:, db * 512:(db + 1) * 512], P.psum[db].a, bc[n][:, db * 512:(db + 1) * 512], ALU.mult)
        P.stt(yt.a, xt[t % 2].a, ALPHA, yt.a, ALU.mult, ALU.add)
        X1 = x1[t % 2]
        ln_tile(P, yt.a, X1.a, st, mv, rstd, epsb)
        P.tt(X1.a, X1.a, bc[2].a, ALU.mult, eng="pool")
        P.tt(X1.a, X1.a, bc[3].a, ALU.add, eng="pool")
        P.dma(x1_d[ts_, :], X1.a, eng="pool")
        ln_tile(P, X1.a, yt.a, st, mv, rstd, epsb)
        hb_ = h2b[t % 2]
        for kc in range(16):
            pb = P.psum[4 + (kc // 4) % 4]
            pv = pb[:, (kc % 4) * 128:(kc % 4 + 1) * 128]
            P.transpose(pv, yt[:, kc * 128:(kc + 1) * 128], identf.a)
            if kc % 4 == 3:
                g4 = kc // 4
                for q in range(4):
                    k2 = g4 * 4 + q
                    P.act(h2f[:, k2, :], pb[:, q * 128:(q + 1) * 128], AF.Identity,
                          bias=modT[:, 48 + k2, n:n + 1], scale=sc2p[:, k2, n:n + 1])
                P.copy(hb_[:, g4 * 4:(g4 + 1) * 4, :], h2f[:, g4 * 4:(g4 + 1) * 4, :], eng="dve")
        P.dma(h2_d[:, :, ts_].re("k p t -> p k t"), hb_.a, eng="pool")
        pr = P.psum[0]
        for kc in range(16):
            P.mm(pr[:, 0:32], h2f[:, kc, :], Wr[:, kc, :], start=(kc == 0), stop=(kc == 15))
        P.tt(lg.a, pr[:, 0:32], rbb.a, ALU.add)
        P.op("dve", lambda e: e.max(m8.ap, lg.ap), [lg.a], [m8.a])
        P.ts(negm.a, m8[:, 0:1], -1.0, ALU.mult)
        P.ts(mk.a, lg.a, m8[:, 3:4], ALU.is_ge)
        P.act(ex.a, lg.a, AF.Exp, bias=negm.a)
        P.tt(ex.a, ex.a, mk.a, ALU.mult)
        P.op("dve", lambda e: e.reduce_sum(ssum.ap, ex.ap, AX.X), [ex.a], [ssum.a])
        P.op("dve", lambda e: e.reciprocal(ssum.ap, ssum.ap), [ssum.a], [ssum.a])
        P.ts(G[:, t, :], ex.a, ssum.a, ALU.mult)
        pg = P.psum[1]
        P.transpose(pg[0:32, 0:128], G[:, t, :], identf.a)
        P.copy(GT[:, ts_], pg[0:32, 0:128], eng="act")
    P.release(m0)
    bcg = P.tile([128, 2048], F32)
    bc2 = [bcg, bcg, P.tile([128, 2048], F32), P.tile([128, 2048], F32)]
    P.dma(bcg.a, V(bc2_d.ap[1].partition_broadcast(128), bc2_d.subs))
    P.dma(bc2[2].a, V(bc2_d.ap[2].partition_broadcast(128), bc2_d.subs))
    P.dma(bc2[3].a, V(bc2_d.ap[3].partition_broadcast(128), bc2_d.subs))
    b2s = P.tile([32, 2048], F32); P.dma(b2s.a, b2_d.a)
    H2g = P.tile([128, 16, 768], BF16)
    acc = P.tile([128, 6, 2048], F32, nsub=6)
    actT = P.tile([128, 6, 768], BF16, nsub=6)
    W1p = [P.tile([128, 16, 256], BF16) for _ in range(2)]
    W2 = P.tile([128, 6, 2048], BF16, nsub=6)
    stg = [P.tile([128, 2048], F32) for _ in range(3)]
    tmps = [dict(xg=P.tile([128, 512], F32), sg=P.tile([128, 512], F32), xl=P.tile([128, 512], F32)) for _ in range(2)]
    xe, ye = stg[0], stg[1]
    kk = [0]
    def load_w1(e, fc, buf):
        for half in range(2):
            s_ = stg[kk[0] % 3]; kk[0] += 1
            sv = s_.a.re("p (a b) -> p a b", b=256)
            P.dma(sv, w1_d[e, fc, :, half * 8:(half + 1) * 8, :])
            P.copy(buf[:, half * 8:(half + 1) * 8, :], sv, eng="pool")
    def load_w2(e):
        for fc in range(6):
            s_ = stg[kk[0] % 3]; kk[0] += 1
            P.dma(s_.a, w2_d[e, :, fc, :])
            P.copy(W2.s(fc), s_.a, eng="pool")
    for (ta, tb) in GROUPS:
        ng = tb - ta
        ntok = ng * 128
        P.dma(H2g[:, :, 0:ntok], h2_d[:, :, ta * 128:tb * 128].re("k p t -> p k t"))
        for ti in range(ng):
            for db in range(4):
                pb = P.psum[4 + db]
                P.mm(pb.a, GT[:, (ta + ti) * 128:(ta + ti + 1) * 128], b2s[:, db * 512:(db + 1) * 512])
                P.copy(acc.s(ti)[:, db * 512:(db + 1) * 512], pb.a, eng=("act" if db % 2 == 0 else "dve"))
        blocks = [(c0, min(c0 + 512, ntok)) for c0 in range(0, ntok, 512)]
        pieces = [(e, fc) for e in range(32) for fc in range(6)]
        load_w1(0, 0, W1p[0])
        for pi_, (e, fc) in enumerate(pieces):
            if pi_ + 1 < len(pieces):
                load_w1(*pieces[pi_ + 1], W1p[(pi_ + 1) % 2])
            if fc == 0:
                load_w2(e)
            Wp = W1p[pi_ % 2]
            for bi, (c0, c1) in enumerate(blocks):
                T = tmps[bi % 2]
                n = c1 - c0
                pg_, pl_ = P.psum[2 * (bi % 2)], P.psum[2 * (bi % 2) + 1]
                for kc in range(16):
                    P.mm(pg_[:, 0:n], Wp[:, kc, 0:128], H2g[:, kc, c0:c1], start=(kc == 0), stop=(kc == 15))
                for kc in range(16):
                    P.mm(pl_[:, 0:n], Wp[:, kc, 128:256], H2g[:, kc, c0:c1], start=(kc == 0), stop=(kc == 15))
                P.ts(T["xg"][:, 0:n], pg_[:, 0:n], b1g[:, e, fc:fc + 1], ALU.add, 7.0, ALU.min)
                P.act(T["sg"][:, 0:n], T["xg"][:, 0:n], AF.Sigmoid, scale=1.702)
                P.ts(T["xl"][:, 0:n], pl_[:, 0:n], b1l[:, e, fc:fc + 1], ALU.add, 7.0, ALU.min)
                P.ts(T["xl"][:, 0:n], T["xl"][:, 0:n], -7.0, ALU.max, 1.0, ALU.add)
                P.tt(T["xg"][:, 0:n], T["xg"][:, 0:n], T["sg"][:, 0:n], ALU.mult)
                P.tt(actT.s(fc)[:, c0:c1], T["xg"][:, 0:n], T["xl"][:, 0:n], ALU.mult)
            if fc == 5:
                for ti in range(ng):
                    for db in range(4):
                        pb = P.psum[4 + db]
                        for f2 in range(6):
                            P.mm(pb.a, actT.s(f2)[:, ti * 128:(ti + 1) * 128], W2.s(f2)[:, db * 512:(db + 1) * 512],
                                 start=(f2 == 0), stop=(f2 == 5))
                        av = acc.s(ti)[:, db * 512:(db + 1) * 512]
                        P.stt(av, pb.a, G[:, ta + ti, e:e + 1], av, ALU.mult, ALU.add)
        for ti in range(ng):
            t = ta + ti
            n = 1 if t == 0 else 0
            ts_ = slice(t * 128, (t + 1) * 128)
            P.dma(xe.a, x1_d[ts_, :])
            P.tt(ye.a, acc.s(ti), bc2[n].a, ALU.mult)
            P.stt(ye.a, xe.a, ALPHA, ye.a, ALU.mult, ALU.add)
            ln_tile(P, ye.a, xe.a, st, mv, rstd, epsb)
            P.tt(xe.a, xe.a, bc2[2].a, ALU.mult, eng="pool")
            P.tt(xe.a, xe.a, bc2[3].a, ALU.add, eng="pool")
            P.dma(out_d[ts_, :], xe.a, eng="pool")
            if t == 0:
                P.dma(bcg.a, V(bc2_d.ap[0].partition_broadcast(128), bc2_d.subs))

def c_decl(P):
    I = "ExternalInput"
    return [P.dram("x", [NT * 128, 2048], F32, I), P.dram("mix", [16, 128, NT * 128], BF16, I),
            P.dram("wout", [128, 16, 2048], F32, I), P.dram("modT", [128, 96, 2], F32, I),
            P.dram("bc1", [4, 2048], F32, I), P.dram("bc2", [4, 2048], F32, I),
            P.dram("wr", [128, 16, 32], F32, I), P.dram("rbb", [128, 32], F32, I),
            P.dram("w1r", [32, 6, 128, 16, 256], F32, I), P.dram("b1g", [128, 32, 6], F32, I), P.dram("b1l", [128, 32, 6], F32, I),
            P.dram("w2r", [32, 128, 6, 2048], F32, I), P.dram("b2", [32, 2048], F32, I), P.dram("identf", [128, 128], F32, I),
            P.dram("x1s", [NT * 128, 2048], F32, "Internal"), P.dram("h2s", [16, 128, NT * 128], BF16, "Internal"),
            P.dram("out", [NT * 128, 2048], F32, "ExternalOutput")]

def c_weights(d, l):
    fm = lambda w: np.ascontiguousarray(w.reshape(w.shape[0] // 128, 128, -1).transpose(1, 0, 2))
    w1 = d['w1'][l]
    w1g = w1[:, :, 0::2].reshape(32, 16, 128, 6, 128); w1l = w1[:, :, 1::2].reshape(32, 16, 128, 6, 128)
    w1r = np.ascontiguousarray(np.concatenate([w1g, w1l], -1).transpose(0, 3, 2, 1, 4))
    b1 = d['b1'][l]
    b1g = np.ascontiguousarray(b1[:, 0::2].reshape(32, 6, 128).transpose(2, 0, 1))
    b1l = np.ascontiguousarray(b1[:, 1::2].reshape(32, 6, 128).transpose(2, 0, 1))
    w2r = np.ascontiguousarray(d['w2'][l].reshape(32, 6, 128, 2048).transpose(0, 2, 1, 3))
    return dict(wout=fm(d['w_out'][l]), wr=fm(d['router_w'][l]), rbb=np.ascontiguousarray(np.broadcast_to(d['router_b'][l], (128, 32))),
                w1r=w1r, b1g=b1g, b1l=b1l, w2r=w2r, b2=np.ascontiguousarray(d['b2'][l]), identf=np.eye(128, dtype=np.float32))

def c_bcasts(d, l, modT):
    def vec(j0, n):
        return np.ascontiguousarray(modT[:, j0:j0 + 16, n].T.reshape(2048))
    B = lambda v: np.ascontiguousarray(v)
    bc1 = np.stack([vec(32, 0), vec(32, 1), B(d['ln1_g'][l]), B(d['ln1_b'][l])])
    bc2 = np.stack([vec(80, 0), vec(80, 1), B(d['ln2_g'][l]), B(d['ln2_b'][l])])
    return bc1, bc2


import ml_dtypes


def ln_tile_a(P, xt, xn, st, mv, rstd, epsb):
    for c in range(4):
        P.op("dve", lambda e, c=c: e.bn_stats(st.ap[:, c, :], xt.ap[:, c * 512:(c + 1) * 512]), [xt], [st.a])
    P.op("dve", lambda e: e.bn_aggr(mv.ap, st.ap.rearrange("p a b -> p (a b)")), [st.a], [mv.a])
    P.act(rstd.a, mv[:, 1:2], AF.Sqrt, bias=epsb.a)
    P.op("dve", lambda e: e.reciprocal(rstd.ap, rstd.ap), [rstd.a], [rstd.a])
    P.ts(xn, xt, mv[:, 0:1], ALU.subtract, rstd.a, ALU.mult)

def build_a():
    nc = bass.Bass("TRN2", target_bir_lowering=False)
    P = Prog(nc)
    x = P.dram("x", [NT * 128, 2048], F32, "ExternalInput")
    cT = P.dram("cT", [128, 16, 2], F32, "ExternalInput")
    wada = P.dram("w_ada", [2048, 12288], F32, "ExternalInput")
    bT = P.dram("bT", [128, 96], F32, "ExternalInput")
    ident_d = P.dram("ident", [128, 128], F32, "ExternalInput")
    hT_o = P.dram("hT", [16, 128, NT * 128], BF16, "ExternalOutput")
    mod_o = P.dram("modT", [128, 96, 2], F32, "ExternalOutput")

    ident = P.tile([128, 128], F32)
    P.dma(ident.a, ident_d.a)
    ct = P.tile([128, 16, 2], F32)
    P.dma(ct.a, cT.a)
    sil = P.tile([128, 16, 2], F32)
    P.act(sil.a, ct.a, AF.Silu)
    bt = P.tile([128, 96], F32)
    P.dma(bt.a, bT.a)
    modT = P.tile([128, 96, 2], F32)
    m = P.mark()
    panels = [P.tile([128, 12288], F32) for _ in range(2)]
    ps = P.psum[0]
    for kc in range(16):
        pn = panels[kc % 2]
        P.dma(pn.a, wada[kc * 128:(kc + 1) * 128, :])
        for j in range(96):
            P.mm(ps[:, 2 * j:2 * j + 2], pn[:, j * 128:(j + 1) * 128], sil[:, kc, :],
                 start=(kc == 0 and j == 0), stop=(kc == 15 and j == 95))
    P.tt(modT.a, ps[:, 0:192].re("p (j n) -> p j n", n=2), bt.a.re("p (j o) -> p j o", o=1).bc([128, 96, 2]), ALU.add)
    P.release(m)
    P.dma(mod_o.a, modT.a)
    sc1p = P.tile([128, 16, 2], F32)
    P.ts(sc1p.a, modT[:, 16:32, :], 1.0, ALU.add)
    xts = [P.tile([128, 2048], F32) for _ in range(2)]
    xns = [P.tile([128, 2048], F32) for _ in range(2)]
    hts = [P.tile([128, 16, 128], BF16) for _ in range(2)]
    st = P.tile([128, 4, 6], F32)
    mv = P.tile([128, 2], F32)
    rstd = P.tile([128, 1], F32)
    epsb = P.tile([128, 1], F32)
    P.memset(epsb.a, EPS)
    for t in range(NT):
        n = 1 if t == 0 else 0
        xt = xts[t % 2]; xn = xns[t % 2]; ht = hts[t % 2]
        P.dma(xt.a, x[t * 128:(t + 1) * 128, :])
        ln_tile_a(P, xt.a, xn.a, st, mv, rstd, epsb)
        for kc in range(16):
            pb = P.psum[1 + (kc // 4) % 4]
            P.transpose(pb[:, (kc % 4) * 128:(kc % 4 + 1) * 128], xn[:, kc * 128:(kc + 1) * 128], ident.a)
            if kc % 2 == 0:
                P.act(ht[:, kc, :], pb[:, (kc % 4) * 128:(kc % 4 + 1) * 128], AF.Identity,
                      bias=modT[:, kc, n:n + 1], scale=sc1p[:, kc, n:n + 1])
            else:
                P.ts(ht[:, kc, :], pb[:, (kc % 4) * 128:(kc % 4 + 1) * 128], sc1p[:, kc, n:n + 1], ALU.mult,
                     modT[:, kc, n:n + 1], ALU.add)
        P.dma(hT_o[:, :, t * 128:(t + 1) * 128].re("k p t -> p k t"), ht.a, eng="pool")
    return P.build()


class Rows:
    def __init__(self, tile, off):
        self.t = tile; self.off = off
    def __getitem__(self, key):
        return self.t[(key[0] + self.off,) + tuple(key[1:])]

def build_b():
    nc = bass.Bass("TRN2", target_bir_lowering=False)
    P = Prog(nc)
    I = "ExternalInput"
    hT_d = P.dram("hT", [16, 128, TOK], BF16, I)
    wf_d = P.dram("wf", [128, 16, 256], F32, I)
    cs_d = P.dram("cs", [128, 256], BF16, I)
    tab_d = P.dram("tab", [8, 32, 128, 2, 512], BF16, I)
    tabc_d = P.dram("tabc", [2, 128, 2, 256], BF16, I)
    hw = hgrn_decl(P)
    mw = mla_decl(P)
    mix_o = P.dram("mix", [8, 128, TOK], BF16, "ExternalOutput")
    fnet(P, hT_d, wf_d, cs_d, tab_d, tabc_d, Rows(mix_o, 0))
    hgrn(P, hT_d, *hw, Rows(mix_o, 2))
    mla(P, hT_d, *mw, Rows(mix_o, 4))
    return P.build()

def build_c():
    nc = bass.Bass("TRN2", target_bir_lowering=False)
    P = Prog(nc)
    ds = c_decl(P)
    phase_c(P, *ds)
    return P.build()

_CACHE = {}

def kernel(**inputs):
    d = {k: np.asarray(v) for k, v in inputs.items()}
    x_cur = d['x'].astype(np.float32).copy()
    ctx_cur = d['ctx'].astype(np.float32).copy()
    c = d['c']; c_ctx = d['c_ctx']
    if 'nc' not in _CACHE:
        _CACHE['nc'] = (build_a(), build_b(), build_c())
        _CACHE['fn'] = fnet_tables()
        _CACHE['hc'] = hgrn_consts()
        _CACHE['rt'] = rope_tables()
    ncA, ncB, ncC = _CACHE['nc']
    tab, tabc, cs = _CACHE['fn']
    identb, maskF, maskB, m01 = _CACHE['hc']
    COS2, SIN2 = _CACHE['rt']
    cores = list(range(8))
    identf = np.eye(128, dtype=np.float32)
    for l in range(4):
        xtoks = []
        in_maps = []
        bT = np.ascontiguousarray(d['b_ada'][l].reshape(96, 128).T)
        for core in cores:
            b, hf = core // 2, core % 2
            xt = np.ascontiguousarray(np.concatenate([ctx_cur[b, hf * 128:(hf + 1) * 128], x_cur[b, hf * 2048:(hf + 1) * 2048]], 0))
            xtoks.append(xt)
            cT = np.ascontiguousarray(np.stack([c[b].reshape(16, 128).T, c_ctx.reshape(16, 128).T], -1))
            in_maps.append({"x": xt, "cT": cT, "w_ada": d['w_ada'][l], "bT": bT, "ident": identf})
        resA = run_bass_kernel_spmd(ncA, in_maps, core_ids=cores).results
        in_maps = []
        wcache = {}
        for hf in range(2):
            w_in = d['w_in'][l]
            wf = np.ascontiguousarray(w_in[:, hf * 256:(hf + 1) * 256].reshape(16, 128, 256).transpose(1, 0, 2))
            m = {"wf": wf, "cs": cs, "tab": tab, "tabc": tabc, "identb": identb, "maskF": maskF, "maskB": maskB, "m01": m01,
                 "cos2": COS2, "sin2": SIN2}
            m.update(hgrn_weights(d, l, hf)); m.update(mla_weights(d, l, hf))
            wcache[hf] = m
        for core in cores:
            b, hf = core // 2, core % 2
            h0 = np.asarray(resA[2 * b]["hT"]); h1 = np.asarray(resA[2 * b + 1]["hT"])
            hfull = np.ascontiguousarray(np.concatenate([h0[:, :, :128], h1[:, :, :128], h0[:, :, 128:], h1[:, :, 128:]], -1))
            m = dict(wcache[hf]); m["hT"] = hfull
            in_maps.append(m)
        resB = run_bass_kernel_spmd(ncB, in_maps, core_ids=cores).results
        in_maps = []
        cw = c_weights(d, l)
        for core in cores:
            b, hf = core // 2, core % 2
            m0 = np.asarray(resB[2 * b]["mix"]); m1 = np.asarray(resB[2 * b + 1]["mix"])
            full = np.concatenate([m0[0:2], m1[0:2], m0[2:4], m1[2:4], m0[4:8], m1[4:8]], 0)
            mc = np.ascontiguousarray(np.concatenate([full[:, :, hf * 128:(hf + 1) * 128],
                                                      full[:, :, 256 + hf * 2048:256 + (hf + 1) * 2048]], -1))
            modT = np.asarray(resA[core]["modT"])
            bc1, bc2 = c_bcasts(d, l, modT)
            m = dict(cw); m.update({"x": xtoks[core], "mix": mc, "modT": modT, "bc1": bc1, "bc2": bc2})
            in_maps.append(m)
        resC = run_bass_kernel_spmd(ncC, in_maps, core_ids=cores).results
        for core in cores:
            b, hf = core // 2, core % 2
            o = np.asarray(resC[core]["out"])
            ctx_cur[b, hf * 128:(hf + 1) * 128] = o[:128]
            x_cur[b, hf * 2048:(hf + 1) * 2048] = o[128:]
    return x_cur.astype(np.float32)
```
